# Optimizing a Trainium2 kernel written in Bass

```python
import jax, jax.numpy as jnp
from jax import lax
import numpy as np


D_MODEL = 1024
BATCH = 8
SEQ = 4096
DEPTH = 1

CTX_LEN = 256
GRID_W = 64
D_RNN = 1024
N_LRU_BLOCKS = 16
LRU_BLOCK = D_RNN // N_LRU_BLOCKS
CONV_W = 4
LRU_C = 8.0
N_HEADS = 16
HEAD_DIM = 64
D_NA = N_HEADS * HEAD_DIM
WIN_R = 8
WIN_C = 16
QBLK_C = 16
KBLK_C = 32
ROPE_BASE = 10000.0
N_EXPERTS = 16
D_EXPERT = 2048
EC_CAPACITY = 2
IN_SIZES = (D_RNN, D_RNN, D_NA, D_NA, D_NA, D_MODEL, D_MODEL)
N_IN = sum(IN_SIZES)
EPS = 1e-6
NEG_INF = -1e30

kernel_name = 'hybrid_rglru_natten_ecmoe_dit_layer'


def rms_norm(x):
    xf = x.astype(jnp.float32)
    return (xf * lax.rsqrt(jnp.mean(xf * xf, axis=-1, keepdims=True) + EPS)).astype(x.dtype)


def modulate(x, shift, scale):
    return x * (1 + scale) + shift


def split_in(z):
    offsets = [int(o) for o in np.cumsum(IN_SIZES)[:-1]]
    return jnp.split(z, offsets, axis=-1)


def centred_dwconv(x, w, b):
    n = x.shape[1]
    left = CONV_W // 2
    xp = jnp.pad(x, ((0, 0), (left, CONV_W - 1 - left), (0, 0)))
    out = b.astype(x.dtype)
    for k in range(CONV_W):
        out = out + xp[:, k:k + n] * w[k]
    return out


def rglru_coeffs(xc, wa, ba, wi, bi, lam):
    B, N, _ = xc.shape
    f32 = jnp.float32
    xf = xc.astype(f32)
    xb = xf.reshape(B, N, N_LRU_BLOCKS, LRU_BLOCK)
    r = jax.nn.sigmoid(jnp.einsum('bngi,gij->bngj', xb, wa.astype(f32)).reshape(B, N, D_RNN) + ba.astype(f32))
    i = jax.nn.sigmoid(jnp.einsum('bngi,gij->bngj', xb, wi.astype(f32)).reshape(B, N, D_RNN) + bi.astype(f32))
    log_a = -LRU_C * r * jax.nn.softplus(-lam.astype(f32))
    a = jnp.exp(log_a)
    b = jnp.sqrt(-jnp.expm1(2.0 * log_a)) * (i * xf)
    return a, b


def _lin_combine(e1, e2):
    a1, b1 = e1
    a2, b2 = e2
    return a1 * a2, a2 * b1 + b2


def linear_recurrence(a, b, h0, reverse):
    A, Bc = lax.associative_scan(_lin_combine, (a, b), axis=1, reverse=reverse)
    return A * h0[:, None, :] + Bc


def axial_rope_angles(n):
    t = jnp.arange(n)
    row = (t // GRID_W).astype(jnp.float32)
    col = (t % GRID_W).astype(jnp.float32)
    n_freq = HEAD_DIM // 4
    inv = ROPE_BASE ** (-jnp.arange(n_freq, dtype=jnp.float32) / n_freq)
    return row[:, None] * inv, col[:, None] * inv


def _rotate(x, ang):
    half = x.shape[-1] // 2
    x1, x2 = x[..., :half], x[..., half:]
    cos = jnp.cos(ang)[:, None, :].astype(x.dtype)
    sin = jnp.sin(ang)[:, None, :].astype(x.dtype)
    return jnp.concatenate([x1 * cos - x2 * sin, x1 * sin + x2 * cos], axis=-1)


def axial_rope(x, ang_r, ang_c):
    h = HEAD_DIM // 2
    return jnp.concatenate([_rotate(x[..., :h], ang_r), _rotate(x[..., h:], ang_c)], axis=-1)


def context_attention(q, k, v):
    s = jnp.einsum('bqhd,bkhd->bhqk', q, k).astype(jnp.float32) * (HEAD_DIM ** -0.5)
    p = jax.nn.softmax(s, axis=-1).astype(v.dtype)
    return jnp.einsum('bhqk,bkhd->bqhd', p, v)


def neighbourhood_attention(q, k, v, k_ctx, v_ctx, rpb):
    B, N, H, Dh = q.shape
    rows = N // GRID_W
    win_r = min(WIN_R, rows)
    n_cb = GRID_W // QBLK_C
    qcols = np.arange(GRID_W).reshape(n_cb, QBLK_C)
    cstart = np.clip(qcols - WIN_C // 2, 0, GRID_W - WIN_C)
    kstart = np.clip(np.arange(n_cb) * QBLK_C - WIN_C // 2, 0, GRID_W - KBLK_C)
    kcols = kstart[:, None] + np.arange(KBLK_C)
    col_ok = (kcols[:, None, :] >= cstart[:, :, None]) & (kcols[:, None, :] < cstart[:, :, None] + WIN_C)
    dc_idx = np.clip(kcols[:, None, :] - qcols[:, :, None], -(WIN_C - 1), WIN_C - 1) + WIN_C - 1
    rpb_c = rpb[:, :, dc_idx]
    scale = Dh ** -0.5
    qg = q.reshape(B, rows, GRID_W, H, Dh)
    kg = k.reshape(B, rows, GRID_W, H, Dh)
    vg = v.reshape(B, rows, GRID_W, H, Dh)
    n_loc = win_r * KBLK_C

    def one_row(r):
        rs = jnp.clip(r - win_r // 2, 0, rows - win_r)
        qr = lax.dynamic_index_in_dim(qg, r, axis=1, keepdims=False).reshape(B, n_cb, QBLK_C, H, Dh)
        kb = lax.dynamic_slice_in_dim(kg, rs, win_r, axis=1)[:, :, kcols]
        vb = lax.dynamic_slice_in_dim(vg, rs, win_r, axis=1)[:, :, kcols]
        s_loc = jnp.einsum('bjqhd,brjkhd->bhjqrk', qr, kb).astype(jnp.float32) * scale
        dr_idx = rs + jnp.arange(win_r) - r + WIN_R - 1
        bias = jnp.take(rpb_c, dr_idx, axis=1).transpose(0, 2, 3, 1, 4).astype(jnp.float32)
        s_loc = jnp.where(col_ok[:, :, None, :], s_loc + bias, NEG_INF)
        s_loc = s_loc.reshape(B, H, n_cb, QBLK_C, n_loc)
        s_ctx = jnp.einsum('bjqhd,bkhd->bhjqk', qr, k_ctx).astype(jnp.float32) * scale
        p = jax.nn.softmax(jnp.concatenate([s_loc, s_ctx], axis=-1), axis=-1).astype(v.dtype)
        p_loc = p[..., :n_loc].reshape(B, H, n_cb, QBLK_C, win_r, KBLK_C)
        o = (jnp.einsum('bhjqrk,brjkhd->bjqhd', p_loc, vb)
             + jnp.einsum('bhjqk,bkhd->bjqhd', p[..., n_loc:], v_ctx))
        return o.reshape(B, GRID_W, H, Dh)

    out = lax.map(one_row, jnp.arange(rows))
    return out.transpose(1, 0, 2, 3, 4).reshape(B, N, H * Dh)


def expert_choice_moe(xn, w_router, w_gate, w_up, w_down):
    B, N, _ = xn.shape
    cap = EC_CAPACITY * N // N_EXPERTS
    logits = jnp.einsum('bnd,de->bne', xn, w_router).astype(jnp.float32)
    aff = jax.nn.softmax(logits, axis=-1)
    g, idx = lax.top_k(aff.transpose(0, 2, 1), cap)
    bidx = jnp.arange(B)[:, None, None]
    xe = xn[bidx, idx]
    hid = jax.nn.silu(jnp.einsum('becd,edf->becf', xe, w_gate)) * jnp.einsum('becd,edf->becf', xe, w_up)
    ye = jnp.einsum('becf,efd->becd', hid, w_down) * g[..., None].astype(xn.dtype)
    return jnp.zeros_like(xn).at[bidx, idx].add(ye)


def setup_inputs(seed: int = 0) -> dict:
    key = jax.random.key(seed)
    ks = jax.random.split(key, 24)
    f32 = jnp.float32

    def nrm(k, shape, fan_in):
        return jax.random.normal(k, shape, f32) * (fan_in ** -0.5)

    def small(k, shape):
        return 0.02 * jax.random.normal(k, shape, f32)

    u = jax.random.uniform(ks[14], (DEPTH, 2, D_RNN), f32, minval=0.9, maxval=0.999)
    p = u ** (1.0 / LRU_C)
    lam = jnp.log(p) - jnp.log1p(-p)
    return {
        'x': jax.random.normal(ks[0], (BATCH, SEQ, D_MODEL), f32),
        'c': jax.random.normal(ks[1], (BATCH, D_MODEL), f32),
        'ctx': jax.random.normal(ks[2], (BATCH, CTX_LEN, D_MODEL), f32),
        'c_ctx': jax.random.normal(ks[3], (D_MODEL,), f32),
        'w_mod': nrm(ks[4], (DEPTH, D_MODEL, 6 * D_MODEL), D_MODEL),
        'b_mod': small(ks[5], (DEPTH, 6 * D_MODEL)),
        'w_in': nrm(ks[6], (DEPTH, D_MODEL, N_IN), D_MODEL),
        'b_in': small(ks[7], (DEPTH, N_IN)),
        'conv_w': nrm(ks[8], (DEPTH, CONV_W, D_RNN), CONV_W),
        'conv_b': small(ks[9], (DEPTH, D_RNN)),
        'lru_wa': nrm(ks[10], (DEPTH, 2, N_LRU_BLOCKS, LRU_BLOCK, LRU_BLOCK), LRU_BLOCK),
        'lru_ba': small(ks[11], (DEPTH, 2, D_RNN)),
        'lru_wi': nrm(ks[12], (DEPTH, 2, N_LRU_BLOCKS, LRU_BLOCK, LRU_BLOCK), LRU_BLOCK),
        'lru_bi': small(ks[13], (DEPTH, 2, D_RNN)),
        'lru_lambda': lam,
        'na_rpb': 0.1 * jax.random.normal(ks[15], (DEPTH, N_HEADS, 2 * WIN_R - 1, 2 * WIN_C - 1), f32),
        'w_proj_rnn': nrm(ks[16], (DEPTH, D_RNN, D_MODEL), D_RNN),
        'w_proj_na': nrm(ks[17], (DEPTH, D_NA, D_MODEL), D_NA),
        'w_out': nrm(ks[18], (DEPTH, D_MODEL, D_MODEL), D_MODEL),
        'w_router': nrm(ks[19], (DEPTH, D_MODEL, N_EXPERTS), D_MODEL),
        'w_exp_gate': nrm(ks[20], (DEPTH, N_EXPERTS, D_MODEL, D_EXPERT), D_MODEL),
        'w_exp_up': nrm(ks[21], (DEPTH, N_EXPERTS, D_MODEL, D_EXPERT), D_MODEL),
        'w_exp_down': nrm(ks[22], (DEPTH, N_EXPERTS, D_EXPERT, D_MODEL), D_EXPERT),
        'final_norm': 1.0 + small(ks[23], (D_MODEL,)),
    }


def reference(x, c, ctx, c_ctx, w_mod, b_mod, w_in, b_in, conv_w, conv_b,
              lru_wa, lru_ba, lru_wi, lru_bi, lru_lambda, na_rpb,
              w_proj_rnn, w_proj_na, w_out, w_router, w_exp_gate, w_exp_up, w_exp_down,
              final_norm):
    B, N, _ = x.shape
    L = ctx.shape[1]
    ang_r, ang_c = axial_rope_angles(N)
    h_ctx = ctx
    for l in range(DEPTH):
        last = l == DEPTH - 1
        mod = (jax.nn.silu(c) @ w_mod[l] + b_mod[l])[:, None, :]
        sh1, sc1, g1, sh2, sc2, g2 = jnp.split(mod, 6, axis=-1)
        modc = (jax.nn.silu(c_ctx) @ w_mod[l] + b_mod[l])[None, None, :]
        csh1, csc1, cg1, csh2, csc2, cg2 = jnp.split(modc, 6, axis=-1)

        xn = modulate(rms_norm(x), sh1, sc1)
        cn = modulate(rms_norm(h_ctx), csh1, csc1)
        yg, xr, q, k, v, gr, gn = split_in(xn @ w_in[l] + b_in[l])
        ycg, xrc, qc, kc, vc, grc, gnc = split_in(cn @ w_in[l] + b_in[l])

        xr = centred_dwconv(xr, conv_w[l], conv_b[l])
        xrc = centred_dwconv(xrc, conv_w[l], conv_b[l])
        h_lat = None
        ctx_states = []
        for d in range(2):
            rev = d == 1
            pa = (lru_wa[l, d], lru_ba[l, d], lru_wi[l, d], lru_bi[l, d], lru_lambda[l, d])
            ac, bc = rglru_coeffs(xrc, *pa)
            hc = linear_recurrence(ac, bc, jnp.zeros_like(ac[:, 0]), rev)
            h_end = hc[:, 0] if rev else hc[:, -1]
            a, b = rglru_coeffs(xr, *pa)
            hd = linear_recurrence(a, b, h_end, rev)
            h_lat = hd if h_lat is None else h_lat + hd
            ctx_states.append(hc)
        y_rnn = h_lat.astype(x.dtype) * jax.nn.gelu(yg)

        q4 = axial_rope(q.reshape(B, N, N_HEADS, HEAD_DIM), ang_r, ang_c)
        k4 = axial_rope(k.reshape(B, N, N_HEADS, HEAD_DIM), ang_r, ang_c)
        v4 = v.reshape(B, N, N_HEADS, HEAD_DIM)
        kc4 = kc.reshape(B, L, N_HEADS, HEAD_DIM)
        vc4 = vc.reshape(B, L, N_HEADS, HEAD_DIM)
        y_na = neighbourhood_attention(q4, k4, v4, kc4, vc4, na_rpb[l])

        mix = jax.nn.sigmoid(gr) * (y_rnn @ w_proj_rnn[l]) + jax.nn.sigmoid(gn) * (y_na @ w_proj_na[l])
        x = x + g1 * (mix @ w_out[l])
        if not last:
            yc_rnn = (ctx_states[0] + ctx_states[1]).astype(x.dtype) * jax.nn.gelu(ycg)
            yc_na = context_attention(qc.reshape(B, L, N_HEADS, HEAD_DIM), kc4, vc4).reshape(B, L, D_NA)
            mixc = (jax.nn.sigmoid(grc) * (yc_rnn @ w_proj_rnn[l])
                    + jax.nn.sigmoid(gnc) * (yc_na @ w_proj_na[l]))
            h_ctx = h_ctx + cg1 * (mixc @ w_out[l])

        xn2 = modulate(rms_norm(x), sh2, sc2)
        x = x + g2 * expert_choice_moe(xn2, w_router[l], w_exp_gate[l], w_exp_up[l], w_exp_down[l])
        if not last:
            cn2 = modulate(rms_norm(h_ctx), csh2, csc2)
            h_ctx = h_ctx + cg2 * expert_choice_moe(cn2, w_router[l], w_exp_gate[l], w_exp_up[l], w_exp_down[l])
    return rms_norm(x) * final_norm
```

```python
import numpy as np
from contextlib import ExitStack
import concourse.bass as bass
import concourse.mybir as mybir
from concourse.bass_utils import run_bass_kernel_spmd
import ml_dtypes

F32 = mybir.dt.float32
BF16 = mybir.dt.bfloat16
I32 = mybir.dt.int32
AF = mybir.ActivationFunctionType
ALU = mybir.AluOpType

D = 1024
NLAT = 4096
NCTX = 256
TE = NLAT + NCTX
NE = 16
DE = 2048
CAP = 512
EPS = 1e-6
NPOOL = 88


class Sem:
    def __init__(self, h, name):
        self.h = h
        self.name = name
        self.n = 0


class Buf:
    def __init__(self, name):
        self.name = name
        self.w = {}
        self.r = {}


class _Rec:
    def __init__(self):
        self.call = None

    def __getattr__(self, name):
        def f(*a, **k):
            self.call = (name, a, k)
        return f


def _capture(fn):
    r = _Rec()
    fn(r)
    assert r.call is not None
    return r.call


class Prog:
    ENG = ["pe", "act", "dve", "pool", "sp"]

    def __init__(self, nc, es):
        self.nc = nc
        self.es = es
        self.q = {e: [] for e in self.ENG}
        self.esem = {e: self.newsem("e_" + e) for e in ["pe", "act", "dve", "pool"]}
        self.waited = {e: {} for e in self.ENG}
        self.dpools = {"sp": [self.newsem("ds%d" % i) for i in range(NPOOL // 2)],
                       "pool": [self.newsem("dp%d" % i) for i in range(NPOOL // 2)]}
        self.dpool = self.dpools["sp"] + self.dpools["pool"]
        self.di = {"sp": 0, "pool": 0}
        self.floor = {}

    def newsem(self, name):
        h = self.es.enter_context(self.nc.semaphore(name))
        return Sem(h, name)

    def _deps(self, eng, reads, writes):
        deps = dict(self.floor)

        def add(d):
            for k, (s, v) in d.items():
                if k not in deps or deps[k][1] < v:
                    deps[k] = (s, v)

        for b in reads:
            add(b.w)
        for b in writes:
            add(b.w)
            add(b.r)
        waits = []
        own = self.esem.get(eng)
        for k, (s, v) in deps.items():
            if s is own and eng == "pe":
                continue
            if self.waited[eng].get(k, 0) >= v:
                continue
            self.waited[eng][k] = v
            waits.append((s, v))
        return waits

    def _mark(self, tok, reads, writes):
        s, v = tok
        for b in reads:
            b.r[id(s)] = (s, v)
        for b in writes:
            b.w[id(s)] = (s, v)

    def op(self, eng, fn, reads=(), writes=()):
        self.group(eng, [fn], reads, writes)

    def group(self, eng, fns, reads=(), writes=()):
        waits = self._deps(eng, reads, writes)
        s = self.esem[eng]
        s.n += 1
        n = len(fns)
        for i, fn in enumerate(fns):
            self.q[eng].append((waits if i == 0 else (), _capture(fn), (s, 1) if i == n - 1 else None))
        self._mark((s, s.n), reads, writes)

    def dma(self, qeng, fn, reads=(), writes=()):
        waits = self._deps(qeng, reads, writes)
        pl = self.dpools[qeng]
        s = pl[self.di[qeng] % len(pl)]
        self.di[qeng] += 1
        if s.n > 0 and self.waited[qeng].get(id(s), 0) < s.n:
            waits.append((s, s.n))
            self.waited[qeng][id(s)] = s.n
        s.n += 16
        self.q[qeng].append((waits, _capture(fn), (s, 16)))
        self._mark((s, s.n), reads, writes)

    def barrier(self):
        for s in list(self.esem.values()) + self.dpool:
            if s.n > 0:
                self.floor[id(s)] = (s, s.n)

    def final_wait(self, eng):
        self.barrier()
        waits = self._deps(eng, (), ())
        self.q[eng].append((waits, None, None))

    def replay(self, eng, e):
        for waits, fn, inc in self.q[eng]:
            for (s, v) in waits:
                e.wait_ge(s.h, v)
            if fn is None:
                continue
            name, a, k = fn
            ins = getattr(e, name)(*a, **k)
            if inc is not None:
                ins.then_inc(inc[0].h, inc[1])


class Arena:
    def __init__(self, ap_f32, nwords):
        self.base = ap_f32
        self.nwords = nwords
        self.total = nwords
        self.off = 0

    def alloc_top(self, nelem, dtype=F32):
        nw = nelem if dtype in (F32, I32) else (nelem + 1) // 2
        nw = (nw + 1) // 2 * 2
        self.nwords -= nw
        assert self.nwords >= self.off
        v = self.base[:, self.nwords:self.nwords + nw]
        return v[:, 0:nelem] if dtype == F32 else v.bitcast(dtype)[:, 0:nelem]

    def mark(self):
        return self.off

    def reset(self, m):
        self.off = m

    def alloc(self, nelem, dtype=F32):
        if dtype == F32 or dtype == I32:
            nw = nelem
        else:
            nw = (nelem + 1) // 2
        nw = (nw + 1) // 2 * 2
        assert self.off + nw <= self.nwords, ("SBUF arena overflow", self.off, nw, self.nwords)
        v = self.base[:, self.off:self.off + nw]
        self.off += nw
        if dtype == F32:
            return v[:, 0:nelem]
        return v.bitcast(dtype)[:, 0:nelem]


def ntiles(n0, n1, step=512):
    out = []
    t = n0
    while t < n1:
        out.append((t, min(t + step, n1)))
        t += step
    return out


def build_nc(stage=99, dbg=False, na_pairs=8, na_rows=64, skip_lru=False):
    nc = bass.Bass("TRN2", target_bir_lowering=False)

    def din(name, shape, dt=F32):
        return nc.dram_tensor(name, list(shape), dt, kind="ExternalInput").ap()

    x_d = din("x", [NLAT, D])
    ctx_d = din("ctx", [NCTX, D])
    cpp_d = din("cpp", [128, 16])
    wmod_d = din("w_mod", [D, 6 * D])
    bmodrow_d = din("bmod_row", [2, 6 * D])
    bmodpp_d = din("bmod_pp", [128, 48])
    win_d = din("w_in", [D, 7 * D])
    binpp_d = din("bin_pp", [128, 56])
    binv_d = din("bin_v", [128, D])
    convw_d = din("convw_pp", [128, 32])
    convb_d = din("convb_pp", [128, 8])
    lruw_d = din("lru_wbd", [2, 2, 8, 128, 128])
    lrub_d = din("lru_b_pp", [128, 32])
    lam_d = din("lam_pp", [128, 16])
    rpbg_d = din("rpbg", [16, 128, 2048])
    maskx_d = din("maskx", [128, 2048])
    wpr_d = din("w_proj_rnn", [D, D])
    wpn_d = din("w_proj_na", [D, D])
    wout_d = din("w_out", [D, D])
    wrt_d = din("wr_pp", [128, 128])
    wg_d = din("w_exp_gate", [NE, D, DE])
    wu_d = din("w_exp_up", [NE, D, DE])
    wd_d = din("w_exp_down", [NE, DE, D])
    fnb_d = din("fn_b", [128, D])
    identf_d = din("ident_f", [128, 128])
    identb_d = din("ident_b", [128, 128], BF16)
    rperm_d = din("rperm", [128, 128], BF16)
    cos_d = din("cos_t", [128, NLAT])
    sin_d = din("sin_t", [128, NLAT])
    iota_d = din("iota_slot", [128, CAP], mybir.dt.uint16)
    tokc_d = din("tokcols", [128, 1024], BF16)
    sel_d = din("sel2", [2, 128])
    g8_d = din("g8", [128, 128])
    l8_d = din("l8", [128, 128])
    wc_d = din("wcomb", [5, 2])
    out_d = nc.dram_tensor("out", [NLAT, D], F32, kind="ExternalOutput").ap()
    yrT_d = nc.dram_tensor("yrT_s", [8, 128, 8, 512], BF16, kind="Internal").ap()
    ynT_d = nc.dram_tensor("ynT_s", [8, 128, 8, 512], BF16, kind="Internal").ap()
    xn2_d = nc.dram_tensor("xn2_s", [NLAT, D], BF16, kind="Internal").ap()
    modb_d = nc.dram_tensor("modb_s", [128, 4096], F32, kind="Internal").ap()
    sg_d = nc.dram_tensor("sg_s", [2, 8, 128, 8, 512], BF16, kind="Internal").ap()
    dbg_d = {}

    def dout(name, shape, dt=F32):
        dbg_d[name] = nc.dram_tensor(name, list(shape), dt, kind="ExternalOutput").ap()
        return dbg_d[name]

    es = ExitStack()
    with es:
        NW = 52800
        arena_t = es.enter_context(nc.sbuf_tensor("arena", [128, NW], F32))
        ar = Arena(arena_t[:, :], NW)
        banks = []
        for i in range(8):
            pt = es.enter_context(nc.psum_tensor("bank%d" % i, [128, 512], F32))
            banks.append(pt[:, :])
        bankb = [Buf("bank%d" % i) for i in range(8)]
        P = Prog(nc, es)

        b_yrT = Buf("yrT")
        b_ynT = Buf("ynT")
        b_xn2d = Buf("xn2d")
        b_out = Buf("out")

        identf = ar.alloc(128)
        identb = ar.alloc(128, BF16)
        mp = ar.alloc(32)
        epsc = ar.alloc(2)
        b_const = Buf("const")
        b_mp = Buf("mp")
        b_modb = Buf("modb")
        P.dma("sp", lambda e: e.dma_start(out=identf, in_=identf_d), writes=[b_const])
        P.dma("sp", lambda e: e.dma_start(out=identb, in_=identb_d), writes=[b_const])
        P.op("dve", lambda e: e.memset(epsc[:, 0:1], EPS), writes=[b_const])
        P.op("dve", lambda e: e.memset(epsc[:, 1:2], 1.0), writes=[b_const])
        m_persist = ar.mark()
        xnT = ar.alloc(8 * TE, BF16)
        xnTv = xnT.rearrange("p (k t) -> p k t", k=8)
        b_xnT = [Buf("xnT%d" % i) for i in range(34)]
        m_xnT = ar.mark()

        def xn_bufs(t0, t1):
            return [b_xnT[i] for i in range(t0 // 128, (t1 + 127) // 128)]
        modb = ar.alloc(4096)
        b_modbd = Buf("modb_d")

        cpp = ar.alloc(16)
        s2 = ar.alloc(16, BF16)
        bmpp = ar.alloc(48)
        modrow = ar.alloc(4096)
        bmrow = ar.alloc(4096)
        sel = ar.alloc(128)
        wm = [ar.alloc(8 * 512, BF16) for _ in range(4)]
        b_cpp, b_s2, b_bm, b_modrow, b_sel = Buf("cpp"), Buf("s2"), Buf("bm"), Buf("modrow"), Buf("sel")
        b_wm = [Buf("wm%d" % i) for i in range(4)]
        P.dma("sp", lambda e: e.dma_start(out=cpp, in_=cpp_d), writes=[b_cpp])
        P.dma("sp", lambda e: e.dma_start(out=bmpp, in_=bmodpp_d), writes=[b_bm])
        P.dma("sp", lambda e: e.dma_start(out=bmrow[0:2, :], in_=bmodrow_d[:, 2048:6144]), writes=[b_bm])
        P.dma("sp", lambda e: e.dma_start(out=sel[0:2, :], in_=sel_d), writes=[b_sel])
        P.op("act", lambda e: e.activation(out=s2, in_=cpp, func=AF.Silu), reads=[b_cpp], writes=[b_s2])
        s2v = s2.rearrange("p (k s) -> p k s", s=2)
        wmod_v = wmod_d.rearrange("(k p) j -> p k j", p=128)
        def mod_load(i):
            sl = i % 4
            wmv = wm[sl].rearrange("p (k j) -> p k j", k=8)
            P.dma("pool", lambda e: e.dma_start(out=wmv, in_=wmod_v[:, :, i * 512:(i + 1) * 512]), writes=[b_wm[sl]])

        def mod_mm(i):
            sl = i % 4
            wmv = wm[sl].rearrange("p (k j) -> p k j", k=8)
            if i < 4:
                for jc in range(4):
                    j = 4 * i + jc
                    fns = []
                    for k in range(8):
                        fns.append(lambda e, k=k, jc=jc, j=j: e.matmul(
                            banks[0][:, 2 * j:2 * j + 2], lhsT=wmv[:, k, jc * 128:(jc + 1) * 128], rhs=s2v[:, k, :],
                            start=(k == 0), stop=(k == 7)))
                    P.group("pe", fns, reads=[b_wm[sl], b_s2], writes=[bankb[0]])
            else:
                bk = 1 + (i % 2)
                fns = []
                for k in range(8):
                    fns.append(lambda e, k=k: e.matmul(
                        banks[bk][0:2, :], lhsT=s2v[:, k, :], rhs=wmv[:, k, :], start=(k == 0), stop=(k == 7)))
                P.group("pe", fns, reads=[b_wm[sl], b_s2], writes=[bankb[bk]])
                o = (i - 4) * 512
                P.op("dve", lambda e: e.tensor_tensor(
                    out=modrow[0:2, o:o + 512], in0=banks[bk][0:2, :], in1=bmrow[0:2, o:o + 512], op=ALU.add),
                    reads=[bankb[bk], b_bm], writes=[b_modrow])

        for i in range(4):
            mod_load(i)
        for i in range(4):
            mod_mm(i)
            mod_load(i + 4)
        mpv = mp.rearrange("p (s j) -> p s j", s=2)
        b0v = banks[0][:, 0:32].rearrange("p (j s) -> p j s", s=2)
        for s_ in range(2):
            P.op("dve", lambda e, s_=s_: e.tensor_tensor(out=mpv[:, s_, :], in0=b0v[:, :, s_], in1=bmpp[:, 0:16], op=ALU.add),
                 reads=[bankb[0], b_bm], writes=[b_mp])
            P.op("dve", lambda e, s_=s_: e.tensor_scalar(out=mpv[:, s_, 8:16], in0=mpv[:, s_, 8:16], scalar1=1.0, scalar2=None,
                                                       op0=ALU.add), reads=[b_mp], writes=[b_mp])
        def mod_finish():
            for n in range(8):
                bk = 3 + (n % 2)
                P.op("pe", lambda e, bk=bk, n=n: e.matmul(banks[bk][:, :], lhsT=sel[0:2, :], rhs=modrow[0:2, n * 512:(n + 1) * 512],
                                                        start=True, stop=True), reads=[b_sel, b_modrow], writes=[bankb[bk]])
                if n in (4, 5):
                    P.op("dve", lambda e, bk=bk, n=n: e.tensor_scalar(out=modb[:, n * 512:(n + 1) * 512], in0=banks[bk][:, :],
                                                                    scalar1=1.0, scalar2=None, op0=ALU.add),
                         reads=[bankb[bk]], writes=[b_modb])
                else:
                    P.op("act", lambda e, bk=bk, n=n: e.activation(out=modb[:, n * 512:(n + 1) * 512], in_=banks[bk][:, :],
                                                                 func=AF.Identity), reads=[bankb[bk]], writes=[b_modb])
            P.dma("sp", lambda e: e.dma_start(out=modb_d, in_=modb), reads=[b_modb], writes=[b_modbd])
            if dbg:
                d_mp = dout("d_mp", [128, 32])
                d_modb = dout("d_modb", [128, 4096])
                P.dma("sp", lambda e: e.dma_start(out=d_mp, in_=mp), reads=[b_mp])
                P.dma("sp", lambda e: e.dma_start(out=d_modb, in_=modb), reads=[b_modb])


        if stage >= 1:
            xin = [ar.alloc(1024) for _ in range(4)]
            xnf = [ar.alloc(1024) for _ in range(4)]
            junk = ar.alloc(1024)
            ss = ar.alloc(34)
            rstd = ar.alloc(34)
            b_xin = [Buf("xin%d" % i) for i in range(4)]
            b_xnf = [Buf("xnf%d" % i) for i in range(4)]
            b_junk = Buf("junk")
            b_ssl = [Buf("ss%d" % i) for i in range(8)]
            def p1_A(ti):
                sl = ti % 4
                b_ss = b_ssl[ti % 8]
                src = ctx_d[ti * 128:(ti + 1) * 128, :] if ti < 2 else x_d[(ti - 2) * 128:(ti - 1) * 128, :]
                P.dma("sp", lambda e: e.dma_start(out=xin[sl], in_=src), writes=[b_xin[sl]])
                P.op("act", lambda e: e.activation(out=junk, in_=xin[sl], func=AF.Square, accum_out=ss[:, ti:ti + 1]),
                     reads=[b_xin[sl]], writes=[b_junk, b_ss])
                P.op("act", lambda e: e.activation(out=rstd[:, ti:ti + 1], in_=ss[:, ti:ti + 1], func=AF.Sqrt,
                                                   bias=epsc[:, 0:1], scale=1.0 / D), reads=[b_ss, b_const], writes=[b_ss])
                P.op("dve", lambda e: e.reciprocal(out=rstd[:, ti:ti + 1], in_=rstd[:, ti:ti + 1]), reads=[b_ss], writes=[b_ss])
                P.op("dve", lambda e: e.tensor_scalar(out=xnf[sl], in0=xin[sl], scalar1=rstd[:, ti:ti + 1], scalar2=None,
                                                     op0=ALU.mult), reads=[b_xin[sl], b_ss], writes=[b_xnf[sl]])

            def p1_B(ti):
                sl = ti % 4
                s_ = 1 if ti < 2 else 0
                bA = 2 * (ti % 4)
                for hb in range(2):
                    bk = bA + hb
                    fns = []
                    for kk in range(4):
                        k = hb * 4 + kk
                        fns.append(lambda e, bk=bk, kk=kk, k=k: e.transpose(
                            banks[bk][:, kk * 128:(kk + 1) * 128], xnf[sl][:, k * 128:(k + 1) * 128], identf))
                    P.group("pe", fns, reads=[b_xnf[sl], b_const], writes=[bankb[bk]])
                    for kk in range(4):
                        k = hb * 4 + kk
                        dst = xnTv[:, k, ti * 128:(ti + 1) * 128]
                        if k % 2 == 0:
                            P.op("act", lambda e, bk=bk, kk=kk, k=k, dst=dst: e.activation(
                                out=dst, in_=banks[bk][:, kk * 128:(kk + 1) * 128], func=AF.Identity,
                                bias=mpv[:, s_, k:k + 1], scale=mpv[:, s_, 8 + k:9 + k]),
                                reads=[bankb[bk], b_mp], writes=[b_xnT[ti]])
                        else:
                            P.op("dve", lambda e, bk=bk, kk=kk, k=k, dst=dst: e.tensor_scalar(
                                out=dst, in0=banks[bk][:, kk * 128:(kk + 1) * 128], scalar1=mpv[:, s_, 8 + k:9 + k],
                                scalar2=mpv[:, s_, k:k + 1], op0=ALU.mult, op1=ALU.add),
                                reads=[bankb[bk], b_mp], writes=[b_xnT[ti]])

            p1_A(0)
            p1_A(1)
            for ti in range(34):
                if ti + 2 < 34:
                    p1_A(ti + 2)
                p1_B(ti)
                if ti % 4 == 1:
                    pi = 4 + ti // 4
                    if pi < 12:
                        mod_mm(pi)
                        if pi + 4 < 12:
                            mod_load(pi + 4)
            mod_finish()
            if dbg:
                d_xnT = dout("d_xnT", [128, 8 * TE], BF16)
                P.dma("sp", lambda e: e.dma_start(out=d_xnT, in_=xnT), reads=b_xnT)
            P.barrier()
            ar.reset(m_xnT)

        win_v = win_d.rearrange("(k p) n -> p k n", p=128)

        if stage >= 2 and not skip_lru:
            m0 = ar.mark()
            cw = ar.alloc(32)
            cb = ar.alloc(8)
            lb = ar.alloc(32)
            lam = ar.alloc(16)
            cneg = ar.alloc(16)
            cneg2 = ar.alloc(16)
            binpp = ar.alloc(56)
            b_lc = Buf("lru_consts")
            for (dst, src) in ((cw, convw_d), (cb, convb_d), (lb, lrub_d), (lam, lam_d), (binpp, binpp_d)):
                P.dma("sp", lambda e, dst=dst, src=src: e.dma_start(out=dst, in_=src), writes=[b_lc])
            P.op("act", lambda e: e.activation(out=cneg, in_=lam, func=AF.Exp, scale=-1.0), reads=[b_lc], writes=[b_lc])
            P.op("act", lambda e: e.activation(out=cneg, in_=cneg, func=AF.Ln, bias=epsc[:, 1:2], scale=1.0), reads=[b_lc, b_const], writes=[b_lc])
            P.op("dve", lambda e: e.tensor_scalar(out=cneg2, in0=cneg, scalar1=-16.0, scalar2=None, op0=ALU.mult), reads=[b_lc], writes=[b_lc])
            P.op("dve", lambda e: e.tensor_scalar(out=cneg, in0=cneg, scalar1=-8.0, scalar2=None, op0=ALU.mult), reads=[b_lc], writes=[b_lc])
            cwv = cw.rearrange("p (j k) -> p j k", k=4)
            lbv = lb.rearrange("p (d a j) -> p d a j", d=2, a=2)
            cnv = cneg.rearrange("p (d j) -> p d j", d=2)
            cn2v = cneg2.rearrange("p (d j) -> p d j", d=2)
            wl = [ar.alloc(8 * 256, BF16), ar.alloc(8 * 256, BF16)]
            wg4 = [ar.alloc(4 * 128, BF16), ar.alloc(4 * 128, BF16)]
            b_wl = [Buf("wl0"), Buf("wl1")]
            b_wg4 = [Buf("wg40"), Buf("wg41")]
            XR = ar.alloc(TE)
            XC = ar.alloc(TE)
            AA = ar.alloc(TE)
            I0 = ar.alloc(TE)
            I1 = ar.alloc(TE)
            XCB = ar.alloc(TE, BF16)
            GYs = [ar.alloc(NLAT, BF16), ar.alloc(NLAT, BF16)]
            b_GYs = [Buf("GY0"), Buf("GY1")]
            XIN = ar.alloc(TE, BF16)
            b_XIN = Buf("XIN")
            _yb = ar.alloc(NLAT, BF16)
            YB = [_yb, _yb]
            b_XR, b_XC, b_AA, b_I0, b_I1, b_XCB = (Buf(n) for n in ("XR", "XC", "AA", "I0", "I1", "XCB"))
            _byb = Buf("YB")
            b_YB = [_byb, _byb]
            IB = [I0, I1]
            b_IB = [b_I0, b_I1]
            etiles = ntiles(0, TE)
            ltiles = ntiles(NCTX, TE)
            lbank = [0]

            def nextbank():
                lbank[0] = (lbank[0] + 1) % 4
                return lbank[0]

            def load_lru_w(j):
                sl = j % 2
                wlv = wl[sl].rearrange("p (k n) -> p k n", k=8)
                P.dma("pool", lambda e: e.dma_start(out=wlv[:, :, 0:128], in_=win_v[:, :, j * 128:(j + 1) * 128]), writes=[b_wl[sl]])
                P.dma("pool", lambda e: e.dma_start(out=wlv[:, :, 128:256], in_=win_v[:, :, D + j * 128:D + (j + 1) * 128]), writes=[b_wl[sl]])
                w4v = wg4[sl].rearrange("p (g m) -> p g m", g=4)
                for d_ in range(2):
                    for a_ in range(2):
                        P.dma("pool", lambda e, d_=d_, a_=a_: e.dma_start(out=w4v[:, d_ * 2 + a_, :], in_=lruw_d[d_, a_, j]), writes=[b_wg4[sl]])

            def in_proj(j):
                sl = j % 2
                wlv = wl[sl].rearrange("p (k n) -> p k n", k=8)
                for (t0, t1) in etiles:
                    bk = nextbank()
                    fns = [(lambda e, k=k, bk=bk, t0=t0, t1=t1: e.matmul(banks[bk][:, 0:t1 - t0], lhsT=wlv[:, k, 128:256], rhs=xnTv[:, k, t0:t1],
                                                                      start=(k == 0), stop=(k == 7))) for k in range(8)]
                    P.group("pe", fns, reads=[b_wl[sl]] + xn_bufs(t0, t1), writes=[bankb[bk]])
                    P.op("act", lambda e, bk=bk, t0=t0, t1=t1: e.activation(out=XIN[:, t0:t1], in_=banks[bk][:, 0:t1 - t0], func=AF.Identity,
                                                                          bias=binpp[:, 8 + j:9 + j], scale=1.0),
                         reads=[bankb[bk], b_lc], writes=[b_XIN])
                for (t0, t1) in ltiles:
                    bk = nextbank()
                    fns = [(lambda e, k=k, bk=bk, t0=t0, t1=t1: e.matmul(banks[bk][:, 0:t1 - t0], lhsT=wlv[:, k, 0:128], rhs=xnTv[:, k, t0:t1],
                                                                      start=(k == 0), stop=(k == 7))) for k in range(8)]
                    P.group("pe", fns, reads=[b_wl[sl]] + xn_bufs(t0, t1), writes=[bankb[bk]])
                    P.op("act", lambda e, bk=bk, t0=t0, t1=t1: e.activation(out=GYs[sl][:, t0 - NCTX:t1 - NCTX], in_=banks[bk][:, 0:t1 - t0],
                                                                          func=AF.Gelu_apprx_tanh, bias=binpp[:, j:j + 1], scale=1.0),
                         reads=[bankb[bk], b_lc], writes=[b_GYs[sl]])

            def gates(j, d_, a_):
                sl = j % 2
                w4v = wg4[sl].rearrange("p (g m) -> p g m", g=4)
                dst = XR if a_ == 0 else IB[d_]
                bdst = b_XR if a_ == 0 else b_IB[d_]
                for (t0, t1) in etiles:
                    bk = nextbank()
                    P.op("pe", lambda e, bk=bk, t0=t0, t1=t1: e.matmul(
                        banks[bk][:, 0:t1 - t0], lhsT=w4v[:, d_ * 2 + a_, :], rhs=XCB[:, t0:t1], start=True, stop=True),
                        reads=[b_wg4[sl], b_XCB], writes=[bankb[bk]])
                    P.op("act", lambda e, bk=bk, t0=t0, t1=t1: e.activation(
                        out=dst[:, t0:t1], in_=banks[bk][:, 0:t1 - t0], func=AF.Sigmoid,
                        bias=lbv[:, d_, a_, j:j + 1], scale=1.0), reads=[bankb[bk], b_lc], writes=[bdst])

            def exps(j, d_):
                P.op("act", lambda e: e.activation(out=AA, in_=XR, func=AF.Exp, scale=cnv[:, d_, j:j + 1]), reads=[b_XR, b_lc], writes=[b_AA])
                P.op("act", lambda e: e.activation(out=XR, in_=XR, func=AF.Exp, scale=cn2v[:, d_, j:j + 1]), reads=[b_XR, b_lc], writes=[b_XR])
                P.op("act", lambda e: e.activation(out=XR, in_=XR, func=AF.Sqrt, bias=epsc[:, 1:2], scale=-1.0), reads=[b_XR, b_const], writes=[b_XR])

            def mulxc(d_):
                Ib, bI = IB[d_], b_IB[d_]
                P.op("dve", lambda e: e.tensor_tensor(out=Ib, in0=Ib, in1=XC, op=ALU.mult), reads=[bI, b_XC], writes=[bI])

            def scan(d_):
                Ib, bI = IB[d_], b_IB[d_]
                P.op("dve", lambda e: e.tensor_tensor(out=Ib, in0=Ib, in1=XR, op=ALU.mult), reads=[bI, b_XR], writes=[bI])
                if d_ == 0:
                    P.op("dve", lambda e: e.tensor_tensor_scan(out=Ib, data0=AA, data1=Ib, initial=0.0, op0=ALU.mult, op1=ALU.add),
                         reads=[bI, b_AA], writes=[bI])
                else:
                    P.op("dve", lambda e: e.tensor_tensor_scan(out=Ib[:, 0:NCTX][:, ::-1], data0=AA[:, 0:NCTX][:, ::-1],
                                                               data1=Ib[:, 0:NCTX][:, ::-1], initial=0.0,
                                                               op0=ALU.mult, op1=ALU.add), reads=[bI, b_AA], writes=[bI])
                    P.op("dve", lambda e: e.tensor_tensor_scan(out=Ib[:, NCTX:TE][:, ::-1], data0=AA[:, NCTX:TE][:, ::-1],
                                                               data1=Ib[:, NCTX:TE][:, ::-1], initial=Ib[:, 0:1],
                                                               op0=ALU.mult, op1=ALU.add), reads=[bI, b_AA], writes=[bI])

            def conv(j):
                P.op("dve", lambda e: e.tensor_scalar(out=XC, in0=XIN, scalar1=cwv[:, j, 2:3], scalar2=cb[:, j:j + 1], op0=ALU.mult, op1=ALU.add),
                     reads=[b_XIN, b_lc], writes=[b_XC])
                for kk in (0, 1, 3):
                    o = kk - 2
                    for (s0, s1) in ((0, NCTX), (NCTX, TE)):
                        a_ = max(s0, s0 - o)
                        b_ = min(s1, s1 - o)
                        P.op("dve", lambda e, a_=a_, b_=b_, o=o, kk=kk: e.scalar_tensor_tensor(
                            out=XC[:, a_:b_], in0=XIN[:, a_ + o:b_ + o], scalar=cwv[:, j, kk:kk + 1], in1=XC[:, a_:b_],
                            op0=ALU.mult, op1=ALU.add), reads=[b_XIN, b_XC, b_lc], writes=[b_XC])
                P.op("act", lambda e: e.activation(out=XCB, in_=XC, func=AF.Identity), reads=[b_XC], writes=[b_XCB])

            load_lru_w(0)
            in_proj(0)
            conv(0)
            gates(0, 0, 0)
            gates(0, 0, 1)
            gates(0, 1, 1)
            for j in range(8):
                sl = j % 2
                nxt = j + 1 < 8
                if nxt:
                    load_lru_w(j + 1)
                exps(j, 0)
                mulxc(0)
                mulxc(1)
                scan(0)
                if nxt:
                    in_proj(j + 1)
                gates(j, 1, 0)
                exps(j, 1)
                if nxt:
                    conv(j + 1)
                scan(1)
                P.op("pool", lambda e: e.tensor_tensor(out=I1[:, NCTX:TE], in0=I0[:, NCTX:TE], in1=I1[:, NCTX:TE], op=ALU.add),
                     reads=[b_I0, b_I1], writes=[b_I1])
                if nxt:
                    gates(j + 1, 0, 0)
                    gates(j + 1, 0, 1)
                P.op("dve", lambda e, sl=sl: e.tensor_tensor(out=YB[sl], in0=I1[:, NCTX:TE], in1=GYs[sl], op=ALU.mult),
                     reads=[b_I1, b_GYs[sl]], writes=[b_YB[sl]])
                P.dma("sp", lambda e, sl=sl: e.dma_start(out=yrT_d[:, :, j, :].rearrange("t p c -> p t c"), in_=YB[sl].rearrange("p (t c) -> p t c", t=8)),
                      reads=[b_YB[sl]], writes=[b_yrT])
                if nxt:
                    gates(j + 1, 1, 1)
            P.barrier()
            ar.reset(m0)

        if stage >= 3:
            m0 = ar.mark()
            cosT = ar.alloc(NLAT)
            sinT = ar.alloc(NLAT)
            rperm = ar.alloc(128, BF16)
            maskx = ar.alloc(2048)
            binpp = ar.alloc(56)
            b_nc = Buf("na_consts")
            for (dst, src) in ((cosT, cos_d), (sinT, sin_d), (rperm, rperm_d), (maskx, maskx_d), (binpp, binpp_d)):
                P.dma("sp", lambda e, dst=dst, src=src: e.dma_start(out=dst, in_=src), writes=[b_nc])
            wq = [ar.alloc(8 * 384, BF16), ar.alloc(8 * 384, BF16)]
            b_wq = [Buf("wq0"), Buf("wq1")]
            rpf = ar.alloc(2048)
            b_rpf = Buf("rpf")
            expb = ar.alloc(4096, BF16)
            expbv = expb.rearrange("p (d h c) -> p d h c", d=8, h=2)
            _bexp = Buf("expb")
            b_expb = [_bexp, _bexp]
            VTs = [ar.alloc(512, BF16), ar.alloc(512, BF16)]
            b_VTs = [Buf("VT0"), Buf("VT1")]
            QFs = [ar.alloc(512, BF16), ar.alloc(512, BF16)]
            b_QFs = [Buf("QF0"), Buf("QF1")]
            T1s = [ar.alloc(512), ar.alloc(512)]
            T2s = [ar.alloc(512), ar.alloc(512)]
            b_T1s = [Buf("T10"), Buf("T11")]
            b_T2s = [Buf("T20"), Buf("T21")]
            QR = ar.alloc(NLAT, BF16)
            KR = ar.alloc(TE, BF16)
            b_QR, b_KR = Buf("QR"), Buf("KR")
            VE = ar.alloc(34 * 130, BF16)
            VO = ar.alloc(31 * 130, BF16)
            b_VE, b_VO = Buf("VE"), Buf("VO")
            VEv = VE.rearrange("p (t h c) -> p t h c", t=34, h=2)
            VOv = VO.rearrange("p (t h c) -> p t h c", t=31, h=2)
            EB = [ar.alloc(768, BF16) for _ in range(3)]
            b_EB = [Buf("EB%d" % i) for i in range(3)]
            YT = [ar.alloc(128), ar.alloc(128)]
            b_YT = [Buf("YT0"), Buf("YT1")]
            RD = ar.alloc(16)
            b_RD = Buf("RD")
            YN = [ar.alloc(NLAT, BF16), ar.alloc(NLAT, BF16)]
            b_YN = [Buf("YN0"), Buf("YN1")]
            P.op("pool", lambda e: e.memset(VE, 1.0), writes=[b_VE])
            P.op("pool", lambda e: e.memset(VO, 1.0), writes=[b_VO])

            def load_na_w(hp):
                sl = hp % 2
                wv_ = wq[sl].rearrange("p (k n) -> p k n", k=8)
                for i_, base in enumerate((2 * D, 3 * D, 4 * D)):
                    P.dma("pool", lambda e, i_=i_, base=base: e.dma_start(out=wv_[:, :, i_ * 128:(i_ + 1) * 128],
                                                                       in_=win_v[:, :, base + hp * 128:base + (hp + 1) * 128]),
                          writes=[b_wq[sl]])

            load_na_w(0)
            ucount = 0
            for hp in range(na_pairs):
                sl = hp % 2
                if hp + 1 < na_pairs:
                    load_na_w(hp + 1)
                wv_ = wq[sl].rearrange("p (k n) -> p k n", k=8)
                qk_tiles = [(0, t0, t1) for (t0, t1) in ntiles(NCTX, TE)] + [(1, t0, t1) for (t0, t1) in [(0, NCTX)] + ntiles(NCTX, TE)]

                def qk_proj(idx):
                    which, t0, t1 = qk_tiles[idx]
                    bk = idx % 2
                    fns = [(lambda e, k=k: e.matmul(banks[bk][:, 0:t1 - t0], lhsT=wv_[:, k, which * 128:(which + 1) * 128], rhs=xnTv[:, k, t0:t1],
                                                    start=(k == 0), stop=(k == 7))) for k in range(8)]
                    P.group("pe", fns, reads=[b_wq[sl]] + xn_bufs(t0, t1), writes=[bankb[bk]])

                def qk_rope(idx):
                    which, t0, t1 = qk_tiles[idx]
                    n = t1 - t0
                    bk = idx % 2
                    bcol = (2 + which) * 8 + hp
                    if which == 1 and t1 <= NCTX:
                        P.op("act", lambda e: e.activation(out=KR[:, t0:t1], in_=banks[bk][:, 0:n], func=AF.Identity, bias=binpp[:, bcol:bcol + 1], scale=1.0),
                             reads=[bankb[bk], b_nc], writes=[b_KR])
                        return
                    qs = idx % 2
                    qf, t1b, t2b = QFs[qs], T1s[qs], T2s[qs]
                    P.op("act", lambda e: e.activation(out=qf[:, 0:n], in_=banks[bk][:, 0:n], func=AF.Identity, bias=binpp[:, bcol:bcol + 1], scale=1.0),
                         reads=[bankb[bk], b_nc], writes=[b_QFs[qs]])
                    bk2 = 2 + (idx % 2)
                    P.op("pe", lambda e: e.matmul(banks[bk2][:, 0:n], lhsT=rperm, rhs=qf[:, 0:n], start=True, stop=True),
                         reads=[b_QFs[qs], b_nc], writes=[bankb[bk2]])
                    l0 = t0 - NCTX
                    P.op("pool", lambda e: e.tensor_tensor(out=t1b[:, 0:n], in0=qf[:, 0:n], in1=cosT[:, l0:l0 + n], op=ALU.mult),
                         reads=[b_QFs[qs], b_nc], writes=[b_T1s[qs]])
                    P.op("dve", lambda e: e.tensor_tensor(out=t2b[:, 0:n], in0=banks[bk2][:, 0:n], in1=sinT[:, l0:l0 + n], op=ALU.mult),
                         reads=[bankb[bk2], b_nc], writes=[b_T2s[qs]])
                    if which == 0:
                        P.op("dve", lambda e: e.tensor_tensor(out=QR[:, l0:l0 + n], in0=t1b[:, 0:n], in1=t2b[:, 0:n], op=ALU.add),
                             reads=[b_T1s[qs], b_T2s[qs]], writes=[b_QR])
                    else:
                        P.op("dve", lambda e: e.tensor_tensor(out=KR[:, t0:t0 + n], in0=t1b[:, 0:n], in1=t2b[:, 0:n], op=ALU.add),
                             reads=[b_T1s[qs], b_T2s[qs]], writes=[b_KR])

                qk_proj(0)
                for idx in range(len(qk_tiles)):
                    if idx + 1 < len(qk_tiles):
                        qk_proj(idx + 1)
                    qk_rope(idx)
                for hh in range(2):
                    h = 2 * hp + hh
                    if not (hh == 0 and hp > 0):
                        P.dma("sp", lambda e, h=h: e.dma_start(out=rpf, in_=rpbg_d[h]), writes=[b_rpf])
                    P.op("act", lambda e: e.activation(out=rpf, in_=rpf, func=AF.Exp), reads=[b_rpf], writes=[b_rpf])
                    P.op("pool", lambda e, hh=hh: e.tensor_tensor(out=expbv[:, :, hh, :], in0=rpf.rearrange("p (d c) -> p d c", d=8),
                                                                 in1=maskx.rearrange("p (d c) -> p d c", d=8), op=ALU.mult),
                         reads=[b_rpf, b_nc], writes=[b_expb[hh]])
                if hp + 1 < na_pairs:
                    P.dma("sp", lambda e: e.dma_start(out=rpf, in_=rpbg_d[2 * (hp + 1)]), writes=[b_rpf])
                bcolv = 4 * 8 + hp
                v_tiles = [(0, NCTX)] + ntiles(NCTX, TE)

                def v_proj(vi):
                    t0, t1 = v_tiles[vi]
                    n = t1 - t0
                    bk = vi % 2
                    vs = vi % 2
                    fns = [(lambda e, k=k: e.matmul(banks[bk][:, 0:n], lhsT=wv_[:, k, 256:384], rhs=xnTv[:, k, t0:t1],
                                                    start=(k == 0), stop=(k == 7))) for k in range(8)]
                    P.group("pe", fns, reads=[b_wq[sl]] + xn_bufs(t0, t1), writes=[bankb[bk]])
                    P.op("act", lambda e: e.activation(out=VTs[vs][:, 0:n], in_=banks[bk][:, 0:n], func=AF.Identity, bias=binpp[:, bcolv:bcolv + 1], scale=1.0),
                         reads=[bankb[bk], b_nc], writes=[b_VTs[vs]])

                def v_tr(vi):
                    t0, t1 = v_tiles[vi]
                    nt4 = (t1 - t0) // 128
                    vs = vi % 2
                    bk2 = 2 + (vi % 2)
                    bkv = banks[bk2].bitcast(BF16)
                    fns = [(lambda e, q=q: e.transpose(bkv[:, q * 128:(q + 1) * 128], VTs[vs][:, q * 128:(q + 1) * 128], identb)) for q in range(nt4)]
                    P.group("pe", fns, reads=[b_VTs[vs], b_const], writes=[bankb[bk2]])
                    ti0 = t0 // 128
                    P.op("dve", lambda e: e.tensor_copy(out=VEv[:, ti0:ti0 + nt4, :, 0:64],
                                                        in_=bkv[:, 0:nt4 * 128].rearrange("p (t h c) -> p t h c", t=nt4, h=2)),
                         reads=[bankb[bk2]], writes=[b_VE])

                v_proj(0)
                for vi in range(len(v_tiles)):
                    if vi + 1 < len(v_tiles):
                        v_proj(vi + 1)
                    v_tr(vi)
                P.dma("sp", lambda e: e.dma_start(out=VO[0:64, :], in_=VE[64:128, 2 * 130:33 * 130]), reads=[b_VE], writes=[b_VO])
                P.dma("sp", lambda e: e.dma_start(out=VO[64:128, :], in_=VE[0:64, 3 * 130:34 * 130]), reads=[b_VE], writes=[b_VO])

                ysl = hp % 2
                b_pvh = [Buf("pvh0"), Buf("pvh1")]
                b_th = [Buf("th0"), Buf("th1")]

                def emit_S(r):
                    rs = min(max(r - 4, 0), 56)
                    dl = r - rs
                    reg = r % 2
                    ebi = r % 3
                    fns = []
                    for kt in range(6):
                        for hh in range(2):
                            pb = hh * 64
                            k0 = NCTX + (rs + 2 * kt) * 64 if kt < 4 else (kt - 4) * 128
                            fns.append(lambda e, kt=kt, hh=hh, reg=reg, k0=k0, pb=pb, r=r: e.matmul(
                                banks[2 * reg + hh][:, kt * 64:(kt + 1) * 64], lhsT=KR[pb:pb + 64, k0:k0 + 128], rhs=QR[pb:pb + 64, r * 64:(r + 1) * 64],
                                start=True, stop=True))
                    P.group("pe", fns, reads=[b_KR, b_QR], writes=[bankb[2 * reg], bankb[2 * reg + 1]])
                    for hh in range(2):
                        P.op("act", lambda e, reg=reg, ebi=ebi, hh=hh: e.activation(out=EB[ebi][:, hh * 384:(hh + 1) * 384],
                                                                                  in_=banks[2 * reg + hh][:, 0:384], func=AF.Exp, scale=0.125),
                             reads=[bankb[2 * reg + hh]], writes=[b_EB[ebi]])
                    ebv = EB[ebi].rearrange("p (h c) -> p h c", h=2)
                    P.op("dve", lambda e, ebv=ebv, dl=dl: e.tensor_tensor(out=ebv[:, :, 0:256], in0=ebv[:, :, 0:256], in1=expbv[:, dl, :, :], op=ALU.mult),
                         reads=[b_EB[ebi], b_expb[0]], writes=[b_EB[ebi]])

                def emit_PV(r):
                    rs = min(max(r - 4, 0), 56)
                    reg = r % 3
                    pvb = 4 + (r % 2)
                    po = 0
                    fns = []
                    for hh in range(2):
                        for kt in range(6):
                            if kt < 4:
                                rv = VEv[:, 2 + rs // 2 + kt, hh, :] if rs % 2 == 0 else VOv[:, (rs - 1) // 2 + kt, hh, :]
                            else:
                                rv = VEv[:, kt - 4, hh, :]
                            fns.append(lambda e, kt=kt, rv=rv, reg=reg, hh=hh, po=po, pvb=pvb: e.matmul(
                                banks[pvb][0:64, po + hh * 128:po + hh * 128 + 65], lhsT=EB[reg][:, hh * 384 + kt * 64:hh * 384 + (kt + 1) * 64], rhs=rv,
                                start=(kt == 0), stop=(kt == 5)))
                    P.group("pe", fns, reads=[b_EB[reg], b_VE] + ([b_VO] if rs % 2 == 1 else []), writes=[bankb[pvb]])
                    pvv = banks[pvb][0:64, po:po + 256].rearrange("p (h c) -> p h c", h=2)
                    rdv = RD[0:64, (r % 4) * 2:(r % 4) * 2 + 2]
                    P.op("dve", lambda e, pvv=pvv, rdv=rdv: e.reciprocal(out=rdv, in_=pvv[:, :, 64]), reads=[bankb[pvb]], writes=[b_RD])
                    for hh in range(2):
                        P.op("dve", lambda e, pvv=pvv, rdv=rdv, hh=hh, r=r: e.tensor_scalar(
                            out=YT[r % 2][0:64, hh * 64:(hh + 1) * 64], in0=pvv[:, hh, 0:64], scalar1=rdv[:, hh:hh + 1], scalar2=None, op0=ALU.mult),
                            reads=[bankb[pvb], b_RD], writes=[b_YT[r % 2]])

                def emit_T(r):
                    tbk = 6 + (r % 2)
                    P.op("pe", lambda e, tbk=tbk, r=r: e.transpose(banks[tbk][:, 0:64], YT[r % 2][0:64, :], identf[0:64, 0:64]),
                         reads=[b_YT[r % 2], b_const], writes=[bankb[tbk]])
                    P.op("act", lambda e, tbk=tbk, r=r: e.activation(out=YN[ysl][:, r * 64:(r + 1) * 64], in_=banks[tbk][:, 0:64], func=AF.Identity),
                         reads=[bankb[tbk]], writes=[b_YN[ysl]])

                emit_S(0)
                for r in range(na_rows):
                    if r + 1 < na_rows:
                        emit_S(r + 1)
                    emit_PV(r)
                    if r >= 1:
                        emit_T(r - 1)
                emit_T(na_rows - 1)
                P.dma("sp", lambda e, ysl=ysl, hp=hp: e.dma_start(out=ynT_d[:, :, hp, :].rearrange("t p c -> p t c"), in_=YN[ysl].rearrange("p (t c) -> p t c", t=8)),
                      reads=[b_YN[ysl]], writes=[b_ynT])
            P.barrier()
            ar.reset(m0)

        b_sgd = Buf("sg_d")
        pre3 = None
        if stage >= 4:
            Wr = ar.alloc_top(8 * D, BF16)
            Wn = ar.alloc_top(8 * D, BF16)
            Wo = ar.alloc_top(8 * D, BF16)
            modb3 = ar.alloc_top(4096)
            b_W = Buf("W3")
            b_modb = Buf("modb3")
            P.dma("sp", lambda e: e.dma_start(out=modb3, in_=modb_d), reads=[b_modbd], writes=[b_modb])
            Wrv = Wr.rearrange("p (k n) -> p k n", k=8)
            Wnv = Wn.rearrange("p (k n) -> p k n", k=8)
            Wov = Wo.rearrange("p (k n) -> p k n", k=8)
            for (dstv, src) in ((Wov, wout_d), (Wrv, wpr_d), (Wnv, wpn_d)):
                P.dma("pool", lambda e, dstv=dstv, src=src: e.dma_start(out=dstv, in_=src.rearrange("(k p) n -> p k n", p=128)), writes=[b_W])
            for k in range(8):
                P.op("dve", lambda e, k=k: e.tensor_tensor(out=Wov[:, k, :], in0=Wov[:, k, :], in1=modb3[:, 0:1024], op=ALU.mult),
                     reads=[b_W, b_modb], writes=[b_W])
            pre3 = True
        if stage >= 3:
            m0 = ar.mark()
            binpp = ar.alloc(56)
            b_c2c = Buf("c2c")
            P.dma("sp", lambda e: e.dma_start(out=binpp, in_=binpp_d), writes=[b_c2c])
            wgc = [ar.alloc(8 * 128, BF16), ar.alloc(8 * 128, BF16)]
            b_wgc = [Buf("wgc0"), Buf("wgc1")]
            SGB = [ar.alloc(NLAT, BF16), ar.alloc(NLAT, BF16)]
            b_SGB = [Buf("SGB0"), Buf("SGB1")]

            def load_gc(u):
                sl = u % 2
                wv = wgc[sl].rearrange("p (k n) -> p k n", k=8)
                P.dma("pool", lambda e: e.dma_start(out=wv, in_=win_v[:, :, 5 * D + u * 128:5 * D + (u + 1) * 128]), writes=[b_wgc[sl]])

            load_gc(0)
            u2 = 0
            for u in range(16):
                sl = u % 2
                if u + 1 < 16:
                    load_gc(u + 1)
                wv = wgc[sl].rearrange("p (k n) -> p k n", k=8)
                for (t0, t1) in ntiles(NCTX, TE):
                    bk = u2 % 4
                    u2 += 1
                    fns = [(lambda e, k=k, bk=bk, t0=t0, t1=t1, wv=wv: e.matmul(banks[bk][:, 0:t1 - t0], lhsT=wv[:, k, :], rhs=xnTv[:, k, t0:t1],
                                                                             start=(k == 0), stop=(k == 7))) for k in range(8)]
                    P.group("pe", fns, reads=[b_wgc[sl]] + xn_bufs(t0, t1), writes=[bankb[bk]])
                    P.op("act", lambda e, bk=bk, t0=t0, t1=t1, sl=sl, u=u: e.activation(
                        out=SGB[sl][:, t0 - NCTX:t1 - NCTX], in_=banks[bk][:, 0:t1 - t0], func=AF.Sigmoid,
                        bias=binpp[:, 40 + u:41 + u], scale=1.0), reads=[bankb[bk], b_c2c], writes=[b_SGB[sl]])
                P.dma("sp", lambda e, sl=sl, u=u: e.dma_start(out=sg_d[u // 8, :, :, u % 8, :].rearrange("t p c -> p t c"), in_=SGB[sl].rearrange("p (t c) -> p t c", t=8)),
                      reads=[b_SGB[sl]], writes=[b_sgd])
            P.barrier()
            ar.reset(m0)

        if dbg and stage >= 2:
            d_yr = dout("d_yrT", [8, 128, 8, 512], BF16)
            P.dma("sp", lambda e: e.dma_start(out=d_yr, in_=yrT_d), reads=[b_yrT])
        if dbg and stage >= 3:
            d_yn = dout("d_ynT", [8, 128, 8, 512], BF16)
            P.dma("sp", lambda e: e.dma_start(out=d_yn, in_=ynT_d), reads=[b_ynT])
            d_sg = dout("d_sg", [2, 8, 128, 8, 512], BF16)
            P.dma("sp", lambda e: e.dma_start(out=d_sg, in_=sg_d), reads=[b_sgd])

        affT = None
        if stage >= 4:
            P.barrier()
            ar.reset(m_persist)
            affT = ar.alloc(32 * 16)
            b_aff = Buf("affT")
            m_aff = ar.mark()
            g1b = modb3[:, 0:1024]
            sh2b = modb3[:, 1024:2048]
            sc2b = modb3[:, 2048:3072]
            wrt = ar.alloc(128)
            b_c3 = Buf("c3")
            P.dma("sp", lambda e: e.dma_start(out=wrt, in_=wrt_d), writes=[b_c3])
            wrtv = wrt.rearrange("p (k n) -> p k n", k=8)
            yr = [ar.alloc(8 * 512, BF16), ar.alloc(8 * 512, BF16)]
            yn = [ar.alloc(8 * 512, BF16), ar.alloc(8 * 512, BF16)]
            sgr = [ar.alloc(8 * 512, BF16), ar.alloc(8 * 512, BF16)]
            sgn = [ar.alloc(8 * 512, BF16), ar.alloc(8 * 512, BF16)]
            b_yr = [Buf("yr0"), Buf("yr1")]
            b_yn = [Buf("yn0"), Buf("yn1")]
            b_sgr = [Buf("sgr0"), Buf("sgr1")]
            b_sgn = [Buf("sgn0"), Buf("sgn1")]
            M1 = ar.alloc(512)
            M2 = ar.alloc(512)
            b_M1, b_M2 = Buf("M1"), Buf("M2")
            MX = ar.alloc(8 * 512, BF16)
            b_MX = Buf("MX")
            MXv = MX.rearrange("p (k t) -> p k t", k=8)
            xt = [ar.alloc(1024), ar.alloc(1024)]
            b_xt = [Buf("xt0"), Buf("xt1")]
            x1 = [ar.alloc(1024), ar.alloc(1024)]
            b_x1 = [Buf("x10"), Buf("x11")]
            xq = [ar.alloc(1024) for _ in range(6)]
            b_xq = [Buf("xq%d" % i) for i in range(6)]
            xqb = [ar.alloc(1024, BF16), ar.alloc(1024, BF16)]
            b_xqb = [Buf("xqb0"), Buf("xqb1")]
            xqT = [ar.alloc(1024), ar.alloc(1024)]
            b_xqT = [Buf("xqT0"), Buf("xqT1")]
            junk = ar.alloc(1024)
            b_junk = Buf("junk3")
            yrT_v = yrT_d
            ynT_v = ynT_d
            sgr_v = sg_d[0]
            sgn_v = sg_d[1]
            stA = ar.alloc(16)
            stB = ar.alloc(16)
            b_stA = [Buf("stA%d" % i) for i in range(8)]
            b_stB = [Buf("stB%d" % i) for i in range(8)]

            def emit_A(tb, s_):
                ti = tb * 4 + s_
                xs = ti % 2
                xqs = ti % 6
                sa = stA[:, (ti % 8) * 2:(ti % 8) * 2 + 2]
                bsa = b_stA[ti % 8]
                P.dma("sp", lambda e: e.dma_start(out=xt[xs], in_=x_d[ti * 128:(ti + 1) * 128, :]), writes=[b_xt[xs]])
                for hf in range(2):
                    ob = 4 + hf
                    fns = [(lambda e, k=k, hf=hf, ob=ob: e.matmul(banks[ob][:, :], lhsT=MXv[:, k, s_ * 128:(s_ + 1) * 128],
                                                              rhs=Wov[:, k, hf * 512:(hf + 1) * 512], start=(k == 0), stop=(k == 7)))
                           for k in range(8)]
                    P.group("pe", fns, reads=[b_MX, b_W], writes=[bankb[ob]])
                    P.op("dve", lambda e, hf=hf, ob=ob: e.tensor_tensor(out=x1[xs][:, hf * 512:(hf + 1) * 512], in0=banks[ob][:, :],
                                                                     in1=xt[xs][:, hf * 512:(hf + 1) * 512], op=ALU.add),
                         reads=[bankb[ob], b_xt[xs]], writes=[b_x1[xs]])
                P.dma("pool", lambda e: e.dma_start(out=out_d[ti * 128:(ti + 1) * 128, :], in_=x1[xs]), reads=[b_x1[xs]], writes=[b_out])
                P.op("act", lambda e: e.activation(out=junk, in_=x1[xs], func=AF.Square, accum_out=sa[:, 0:1]),
                     reads=[b_x1[xs]], writes=[b_junk, bsa])
                P.op("act", lambda e: e.activation(out=sa[:, 1:2], in_=sa[:, 0:1], func=AF.Sqrt, bias=epsc[:, 0:1], scale=1.0 / D),
                     reads=[bsa, b_const], writes=[bsa])
                P.op("dve", lambda e: e.reciprocal(out=sa[:, 1:2], in_=sa[:, 1:2]), reads=[bsa], writes=[bsa])
                P.op("dve", lambda e: e.scalar_tensor_tensor(out=xq[xqs], in0=x1[xs], scalar=sa[:, 1:2], in1=sc2b, op0=ALU.mult, op1=ALU.mult),
                     reads=[b_x1[xs], bsa, b_modb], writes=[b_xq[xqs]])
                P.op("pool", lambda e: e.tensor_tensor(out=xq[xqs], in0=xq[xqs], in1=sh2b, op=ALU.add), reads=[b_xq[xqs], b_modb], writes=[b_xq[xqs]])
                P.op("act", lambda e: e.activation(out=xqb[xs], in_=xq[xqs], func=AF.Identity), reads=[b_xq[xqs]], writes=[b_xqb[xs]])
                P.dma("pool", lambda e: e.dma_start(out=xn2_d[ti * 128:(ti + 1) * 128, :], in_=xqb[xs]), reads=[b_xqb[xs]], writes=[b_xn2d])

            def emit_B(ti):
                xqs = ti % 6
                ts_ = ti % 2
                sb_ = stB[:, (ti % 8) * 2:(ti % 8) * 2 + 2]
                bsb = b_stB[ti % 8]
                xT = xqT[ts_]
                xTv = xT.rearrange("p (k t) -> p k t", k=8)
                for hb in range(2):
                    bk = 6 + hb
                    fns = [(lambda e, kk=kk, hb=hb, bk=bk: e.transpose(banks[bk][:, kk * 128:(kk + 1) * 128],
                                                                    xq[xqs][:, (hb * 4 + kk) * 128:(hb * 4 + kk + 1) * 128], identf))
                           for kk in range(4)]
                    P.group("pe", fns, reads=[b_xq[xqs], b_const], writes=[bankb[bk]])
                    if hb == 0:
                        P.op("act", lambda e, bk=bk: e.activation(out=xT[:, 0:512], in_=banks[bk][:, :], func=AF.Identity), reads=[bankb[bk]], writes=[b_xqT[ts_]])
                    else:
                        P.op("dve", lambda e, bk=bk: e.tensor_copy(out=xT[:, 512:1024], in_=banks[bk][:, :]), reads=[bankb[bk]], writes=[b_xqT[ts_]])
                fns = [(lambda e, k=k: e.matmul(banks[6][:, 0:16], lhsT=xTv[:, k, :], rhs=wrtv[:, k, :], start=(k == 0), stop=(k == 7))) for k in range(8)]
                P.group("pe", fns, reads=[b_xqT[ts_], b_c3], writes=[bankb[6]])
                P.op("act", lambda e: e.activation(out=affT[:, ti * 16:(ti + 1) * 16], in_=banks[6][:, 0:16], func=AF.Exp, accum_out=sb_[:, 0:1]),
                     reads=[bankb[6]], writes=[b_aff, bsb])
                P.op("dve", lambda e: e.reciprocal(out=sb_[:, 1:2], in_=sb_[:, 0:1]), reads=[bsb], writes=[bsb])
                P.op("dve", lambda e: e.tensor_scalar(out=affT[:, ti * 16:(ti + 1) * 16], in0=affT[:, ti * 16:(ti + 1) * 16], scalar1=sb_[:, 1:2],
                                                     scalar2=None, op0=ALU.mult), reads=[b_aff, bsb], writes=[b_aff])

            def load_blk(tb):
                sl = tb % 2
                T0 = tb * 512
                for (buf, bb, src, bsrc) in ((yr[sl], b_yr[sl], yrT_v, b_yrT), (yn[sl], b_yn[sl], ynT_v, b_ynT),
                                             (sgr[sl], b_sgr[sl], sgr_v, b_sgd), (sgn[sl], b_sgn[sl], sgn_v, b_sgd)):
                    bv = buf.rearrange("p (k t) -> p k t", k=8)
                    P.dma("sp", lambda e, bv=bv, src=src: e.dma_start(out=bv, in_=src[tb]), reads=[bsrc], writes=[bb])

            u3 = 0
            for tb in range(8):
                sl = tb % 2
                T0 = tb * 512
                yrv = yr[sl].rearrange("p (k t) -> p k t", k=8)
                ynv = yn[sl].rearrange("p (k t) -> p k t", k=8)
                srv = sgr[sl].rearrange("p (k t) -> p k t", k=8)
                snv = sgn[sl].rearrange("p (k t) -> p k t", k=8)
                if tb == 0:
                    load_blk(0)
                if tb + 1 < 8:
                    load_blk(tb + 1)
                for c in range(8):
                    for br in range(2):
                        ysrc = yrv if br == 0 else ynv
                        bys = b_yr[sl] if br == 0 else b_yn[sl]
                        gsrc = srv if br == 0 else snv
                        bgs = b_sgr[sl] if br == 0 else b_sgn[sl]
                        Wp = Wrv if br == 0 else Wnv
                        zb = 0 + (u3 % 4)
                        u3 += 1
                        fns = [(lambda e, k=k, zb=zb, Wp=Wp, ysrc=ysrc, c=c: e.matmul(banks[zb][:, :], lhsT=Wp[:, k, c * 128:(c + 1) * 128], rhs=ysrc[:, k, :],
                                                                                  start=(k == 0), stop=(k == 7))) for k in range(8)]
                        P.group("pe", fns, reads=[b_W, bys], writes=[bankb[zb]])
                        Mx = M1 if br == 0 else M2
                        bMx = b_M1 if br == 0 else b_M2
                        P.op("dve", lambda e, zb=zb, gsrc=gsrc, c=c, Mx=Mx: e.tensor_tensor(out=Mx, in0=banks[zb][:, :], in1=gsrc[:, c, :], op=ALU.mult),
                             reads=[bankb[zb], bgs], writes=[bMx])
                    P.op("pool", lambda e, c=c: e.tensor_tensor(out=MXv[:, c, :], in0=M1, in1=M2, op=ALU.add),
                         reads=[b_M1, b_M2], writes=[b_MX])
                    if tb >= 1 and c % 2 == 1:
                        emit_B((tb - 1) * 4 + c // 2)
                for s_ in range(4):
                    emit_A(tb, s_)
            for ti in range(28, 32):
                emit_B(ti)
            if dbg:
                d_aff = dout("d_affT", [128, 512])
                P.dma("sp", lambda e: e.dma_start(out=d_aff, in_=affT), reads=[b_aff])
            P.barrier()
            ar.reset(m_aff)
            ar.nwords = ar.total

        if stage >= 6:
            WG = [ar.alloc_top(8 * 1024, BF16), ar.alloc_top(8 * 1024, BF16)]
            WU = [ar.alloc_top(8 * 1024, BF16), ar.alloc_top(8 * 1024, BF16)]
            WD = [ar.alloc_top(8 * 1024, BF16), ar.alloc_top(8 * 1024, BF16), ar.alloc_top(8 * 1024, BF16)]
            b_WG = [Buf("WG0"), Buf("WG1")]
            b_WD = [Buf("WD%d" % i) for i in range(3)]

            def load_g(ex, half):
                sl = (ex * 2 + half) % 2
                wgv = WG[sl].rearrange("p (k n) -> p k n", k=8)
                wuv = WU[sl].rearrange("p (k n) -> p k n", k=8)
                P.dma("pool", lambda e: e.dma_start(out=wgv, in_=wg_d[ex].rearrange("(k p) f -> p k f", p=128)[:, :, half * 1024:(half + 1) * 1024]),
                      writes=[b_WG[sl]])
                P.dma("pool", lambda e: e.dma_start(out=wuv, in_=wu_d[ex].rearrange("(k p) f -> p k f", p=128)[:, :, half * 1024:(half + 1) * 1024]),
                      writes=[b_WG[sl]])

            def load_d(ex, half):
                sl = (ex * 2 + half) % 3
                wdv = WD[sl].rearrange("p (f n) -> p f n", f=8)
                P.dma("pool", lambda e: e.dma_start(out=wdv, in_=wd_d[ex].rearrange("(f p) n -> p f n", p=128)[:, half * 8:(half + 1) * 8, :]),
                      writes=[b_WD[sl]])

            load_g(0, 0)
            load_g(0, 1)
            load_d(0, 0)
            load_d(0, 1)
        if stage >= 5:
            m4 = ar.mark()
            idxf = ar.alloc(64)
            idxi = ar.alloc(64, I32)
            gsl = ar.alloc(64)
            b_idx = Buf("idx")
            m_idx = ar.mark()
            codeT = ar.alloc(512)
            b_code = Buf("codeT")
            cols = ar.alloc(32 * 16 * 5, BF16)
            b_cols = Buf("cols")
            colsv = cols.rearrange("p (t e c) -> p t e c", t=32, e=16)
            iot = ar.alloc(CAP, mybir.dt.uint16)
            wcomb = ar.alloc(2)
            PM = [ar.alloc(CAP, BF16) for _ in range(6)]
            b_PM = [Buf("PM%d" % i) for i in range(6)]
            RS = ar.alloc(CAP)
            b_RS = Buf("RS")
            m_keep = ar.mark()
            tokc = ar.alloc(1024, BF16)
            b_c4 = Buf("c4")
            P.dma("sp", lambda e: e.dma_start(out=tokc, in_=tokc_d), writes=[b_c4])
            P.dma("sp", lambda e: e.dma_start(out=iot, in_=iota_d), writes=[b_c4])
            P.dma("sp", lambda e: e.dma_start(out=wcomb[0:5, :], in_=wc_d), writes=[b_c4])
            tokcv = tokc.rearrange("p (t e c) -> p t e c", t=32, e=16)
            affTv = affT.rearrange("p (t e) -> p t e", e=16)
            AT = ar.alloc(512)
            A8 = ar.alloc(512)
            CM8 = ar.alloc(512)
            CU8 = ar.alloc(512)
            ON8 = ar.alloc(512)
            Gm = ar.alloc(128)
            Lm = ar.alloc(128)
            b8 = ar.alloc(16)
            b_AT, b_A8, b_CM8, b_CU8, b_ON8, b_GL, b_b8 = (Buf(n) for n in ("AT", "A8", "CM8", "CU8", "ON8", "GL", "b8"))
            P.dma("sp", lambda e: e.dma_start(out=Gm, in_=g8_d), writes=[b_GL])
            P.dma("sp", lambda e: e.dma_start(out=Lm, in_=l8_d), writes=[b_GL])
            lo8, mid8, tmp8 = b8[:, 0:1], b8[:, 1:2], b8[:, 4:5]
            cnt8 = b8[:, 2:4]
            P.op("dve", lambda e: e.memset(b8, 0.0), writes=[b_b8])
            P.op("pool", lambda e: e.memset(ON8, 1.0), writes=[b_ON8])
            P.op("dve", lambda e: e.tensor_copy(out=AT.rearrange("p (q e s) -> p q e s", q=4, e=16),
                                                in_=affT.rearrange("p (s q e) -> p q e s", s=8, q=4)), reads=[b_aff], writes=[b_AT])
            fns = [(lambda e, q=q: e.transpose(banks[0][:, q * 128:(q + 1) * 128], AT[:, q * 128:(q + 1) * 128], identf)) for q in range(4)]
            P.group("pe", fns, reads=[b_AT, b_const], writes=[bankb[0]])
            P.op("act", lambda e: e.activation(out=A8, in_=banks[0][:, :], func=AF.Identity), reads=[bankb[0]], writes=[b_A8])
            for it in range(25):
                wk = 2.0 ** (-(it + 1))
                P.op("dve", lambda e, wk=wk: e.tensor_scalar(out=mid8, in0=lo8, scalar1=wk, scalar2=None, op0=ALU.add), reads=[b_b8], writes=[b_b8])
                P.op("dve", lambda e: e.tensor_scalar(out=CM8, in0=A8, scalar1=mid8, scalar2=0.0, op0=ALU.is_ge, op1=ALU.add, accum_out=cnt8[:, 0:1]),
                     reads=[b_A8, b_b8], writes=[b_CM8, b_b8])
                P.op("pe", lambda e: e.matmul(banks[1][:, 0:2], lhsT=Gm, rhs=cnt8, start=True, stop=True), reads=[b_GL, b_b8], writes=[bankb[1]])
                P.op("dve", lambda e, wk=wk: e.tensor_scalar(out=tmp8, in0=banks[1][:, 0:1], scalar1=float(CAP) - 0.5, scalar2=wk, op0=ALU.is_ge, op1=ALU.mult),
                     reads=[bankb[1]], writes=[b_b8])
                P.op("dve", lambda e: e.tensor_tensor(out=lo8, in0=lo8, in1=tmp8, op=ALU.add), reads=[b_b8], writes=[b_b8])
            P.op("dve", lambda e: e.tensor_scalar(out=CM8, in0=A8, scalar1=lo8, scalar2=0.0, op0=ALU.is_ge, op1=ALU.add, accum_out=cnt8[:, 0:1]),
                 reads=[b_A8, b_b8], writes=[b_CM8, b_b8])
            P.op("dve", lambda e: e.tensor_tensor_scan(out=CU8, data0=ON8, data1=CM8, initial=0.0, op0=ALU.mult, op1=ALU.add),
                 reads=[b_CM8, b_ON8], writes=[b_CU8])
            P.op("pe", lambda e: e.matmul(banks[1][:, 0:2], lhsT=Lm, rhs=cnt8, start=True, stop=True), reads=[b_GL, b_b8], writes=[bankb[1]])
            P.op("dve", lambda e: e.tensor_copy(out=tmp8, in_=banks[1][:, 0:1]), reads=[bankb[1]], writes=[b_b8])
            P.op("dve", lambda e: e.scalar_tensor_tensor(out=CU8, in0=CU8, scalar=tmp8, in1=CM8, op0=ALU.add, op1=ALU.mult),
                 reads=[b_CU8, b_CM8, b_b8], writes=[b_CU8])
            P.op("dve", lambda e: e.tensor_scalar(out=CU8, in0=CU8, scalar1=-1.0, scalar2=None, op0=ALU.add), reads=[b_CU8], writes=[b_CU8])
            fns = [(lambda e, q=q: e.transpose(banks[2][:, q * 128:(q + 1) * 128], CU8[:, q * 128:(q + 1) * 128], identf)) for q in range(4)]
            P.group("pe", fns, reads=[b_CU8, b_const], writes=[bankb[2]])
            P.op("dve", lambda e: e.tensor_copy(out=codeT.rearrange("p (s q e) -> p q e s", s=8, q=4),
                                                in_=banks[2][:, :].rearrange("p (q e s) -> p q e s", q=4, e=16)), reads=[bankb[2]], writes=[b_code])
            codeTv = codeT.rearrange("p (t e) -> p t e", e=16)
            for c_ in range(2):
                P.op("dve", lambda e, c_=c_: e.tensor_copy(out=colsv[:, :, :, c_], in_=tokcv[:, :, :, c_]),
                     reads=[b_c4], writes=[b_cols])
            R1 = ar.alloc(512)
            R2 = ar.alloc(512)
            b_R = Buf("R12")
            R1v = R1.rearrange("p (t e) -> p t e", e=16)
            R2v = R2.rearrange("p (t e) -> p t e", e=16)
            P.op("dve", lambda e: e.tensor_copy(out=colsv[:, :, :, 2], in_=affTv), reads=[b_aff], writes=[b_cols])
            P.op("dve", lambda e: e.tensor_tensor(out=R1v, in0=affTv, in1=colsv[:, :, :, 2], op=ALU.subtract), reads=[b_aff, b_cols], writes=[b_R])
            P.op("dve", lambda e: e.tensor_copy(out=colsv[:, :, :, 3], in_=R1v), reads=[b_R], writes=[b_cols])
            P.op("dve", lambda e: e.tensor_tensor(out=R2v, in0=R1v, in1=colsv[:, :, :, 3], op=ALU.subtract), reads=[b_R, b_cols], writes=[b_R])
            P.op("dve", lambda e: e.tensor_copy(out=colsv[:, :, :, 4], in_=R2v), reads=[b_R], writes=[b_cols])
            idxfv = idxf.rearrange("p (e s) -> p e s", s=4)
            gslv = gsl.rearrange("p (e s) -> p e s", s=4)
            idxiv = idxi.rearrange("p (e s) -> p e s", s=4)
            b_idxe = [Buf("idx%d" % i) for i in range(NE)]

            def idx_dve(ex, ti):
                pm = (ex * 32 + ti) % 6
                P.op("dve", lambda e: e.tensor_scalar(out=PM[pm], in0=iot, scalar1=codeTv[:, ti, ex:ex + 1], scalar2=None, op0=ALU.is_equal),
                     reads=[b_c4, b_code], writes=[b_PM[pm]])

            def idx_pe(ex, ti):
                pm = (ex * 32 + ti) % 6
                P.op("pe", lambda e: e.matmul(banks[0][0:5, :], lhsT=colsv[:, ti, ex, :], rhs=PM[pm], start=(ti == 0), stop=(ti == 31)),
                     reads=[b_cols, b_PM[pm]], writes=[bankb[0]])

            def idx_fin(ex):
                P.op("act", lambda e: e.activation(out=RS[0:5, :], in_=banks[0][0:5, :], func=AF.Identity), reads=[bankb[0]], writes=[b_RS])
                fns = [(lambda e, s_=s_: e.matmul(banks[1][:, 2 * s_:2 * s_ + 2], lhsT=RS[0:5, s_ * 128:(s_ + 1) * 128], rhs=wcomb[0:5, :],
                                                  start=True, stop=True)) for s_ in range(4)]
                P.group("pe", fns, reads=[b_RS, b_c4], writes=[bankb[1]])
                b2v = banks[1][:, 0:8].rearrange("p (s c) -> p s c", c=2)
                P.op("dve", lambda e: e.tensor_copy(out=idxfv[:, ex, :], in_=b2v[:, :, 0]), reads=[bankb[1]], writes=[b_idxe[ex]])
                P.op("dve", lambda e: e.tensor_copy(out=gslv[:, ex, :], in_=b2v[:, :, 1]), reads=[bankb[1]], writes=[b_idxe[ex]])
                P.op("dve", lambda e: e.tensor_copy(out=idxiv[:, ex, :], in_=idxfv[:, ex, :]), reads=[b_idxe[ex]], writes=[b_idxe[ex]])

            n_pre = NE if stage < 6 else 2
            for ex in range(n_pre):
                for ti in range(32):
                    idx_dve(ex, ti)
                    idx_pe(ex, ti)
                idx_fin(ex)
            if dbg:
                d_idx = dout("d_idx", [128, 64])
                d_gsl = dout("d_gsl", [128, 64])
                d_code = dout("d_codeT", [128, 512])
                P.dma("sp", lambda e: e.dma_start(out=d_idx, in_=idxf), reads=b_idxe)
                P.dma("sp", lambda e: e.dma_start(out=d_gsl, in_=gsl), reads=b_idxe)
                P.dma("sp", lambda e: e.dma_start(out=d_code, in_=codeT), reads=[b_code])
            P.barrier()
            ar.reset(m_keep)

        if stage >= 6:
            g2b = ar.alloc(1024)
            b_modb = Buf("g2b")
            P.dma("sp", lambda e: e.dma_start(out=g2b, in_=modb_d[:, 3072:4096]), reads=[b_modbd], writes=[b_modb])
            XE = [ar.alloc(4 * D, BF16), ar.alloc(4 * D, BF16)]
            b_XE = [Buf("XE0"), Buf("XE1")]
            XT = [ar.alloc(8 * CAP, BF16), ar.alloc(8 * CAP, BF16)]
            b_XT = [Buf("XT0"), Buf("XT1")]
            HT = ar.alloc(16 * CAP, BF16)
            b_HT = Buf("HT")
            HTv = HT.rearrange("p (f t) -> p f t", f=16)
            SGx = [ar.alloc(CAP), ar.alloc(CAP)]
            b_SGx = [Buf("SGx0"), Buf("SGx1")]
            YE = [ar.alloc(D), ar.alloc(D)]
            b_YE = [Buf("YE0"), Buf("YE1")]

            def gather(ex):
                sl = ex % 2
                xev = XE[sl].rearrange("p (s d) -> p s d", s=4)
                for s_ in range(4):
                    P.dma("pool", lambda e, s_=s_: e.indirect_dma_start(out=xev[:, s_, :], out_offset=None, in_=xn2_d[:, :],
                                                                      in_offset=bass.IndirectOffsetOnAxis(ap=idxiv[:, ex, s_:s_ + 1], axis=0)),
                          reads=[b_xn2d, b_idxe[ex]], writes=[b_XE[sl]])

            gather(0)
            u5 = 0
            for ex in range(NE):
                sl = ex % 2
                xev = XE[sl].rearrange("p (s d) -> p s d", s=4)
                xtv = XT[sl].rearrange("p (k t) -> p k t", k=8)
                for k in range(8):
                    bk = k % 2
                    bkv = banks[bk].bitcast(BF16)
                    fns = [(lambda e, bkv=bkv, s_=s_, k=k: e.transpose(bkv[:, s_ * 128:(s_ + 1) * 128], xev[:, s_, k * 128:(k + 1) * 128], identb))
                           for s_ in range(4)]
                    P.group("pe", fns, reads=[b_XE[sl], b_const], writes=[bankb[bk]])
                    if k % 2 == 0:
                        P.op("act", lambda e, bkv=bkv, k=k: e.activation(out=xtv[:, k, :], in_=bkv[:, 0:512], func=AF.Identity), reads=[bankb[bk]], writes=[b_XT[sl]])
                    else:
                        P.op("dve", lambda e, bkv=bkv, k=k: e.tensor_copy(out=xtv[:, k, :], in_=bkv[:, 0:512]), reads=[bankb[bk]], writes=[b_XT[sl]])
                if ex + 1 < NE:
                    gather(ex + 1)
                for half in range(2):
                    gs = (ex * 2 + half) % 2
                    wgv = WG[gs].rearrange("p (k n) -> p k n", k=8)
                    wuv = WU[gs].rearrange("p (k n) -> p k n", k=8)
                    for fc in range(8):
                        f = half * 8 + fc
                        gb = 2 + (u5 % 2)
                        ub = 4 + (u5 % 2)
                        sgi = u5 % 2
                        u5 += 1
                        fns = [(lambda e, k=k, gb=gb, fc=fc, wgv=wgv: e.matmul(banks[gb][:, :], lhsT=wgv[:, k, fc * 128:(fc + 1) * 128], rhs=xtv[:, k, :],
                                                                           start=(k == 0), stop=(k == 7))) for k in range(8)]
                        P.group("pe", fns, reads=[b_WG[gs], b_XT[sl]], writes=[bankb[gb]])
                        fns = [(lambda e, k=k, ub=ub, fc=fc, wuv=wuv: e.matmul(banks[ub][:, :], lhsT=wuv[:, k, fc * 128:(fc + 1) * 128], rhs=xtv[:, k, :],
                                                                           start=(k == 0), stop=(k == 7))) for k in range(8)]
                        P.group("pe", fns, reads=[b_WG[gs], b_XT[sl]], writes=[bankb[ub]])
                        nxe = ex + 2
                        if nxe < NE:
                            idx_dve(nxe, 2 * f)
                            idx_dve(nxe, 2 * f + 1)
                        P.op("act", lambda e, gb=gb, sgi=sgi: e.activation(out=SGx[sgi], in_=banks[gb][:, :], func=AF.Silu), reads=[bankb[gb]], writes=[b_SGx[sgi]])
                        P.op("dve", lambda e, ub=ub, sgi=sgi, f=f: e.tensor_tensor(out=HTv[:, f, :], in0=banks[ub][:, :], in1=SGx[sgi], op=ALU.mult),
                             reads=[bankb[ub], b_SGx[sgi]], writes=[b_HT])
                        if nxe < NE:
                            idx_pe(nxe, 2 * f)
                            idx_pe(nxe, 2 * f + 1)
                    nx = ex * 2 + half + 2
                    if nx < 2 * NE:
                        load_g(nx // 2, nx % 2)
                if ex + 2 < NE:
                    idx_fin(ex + 2)
                for s_ in range(4):
                    ysl = u5 % 2
                    for hf in range(2):
                        ob = 6 + hf
                        fns = []
                        for f in range(16):
                            ds = (ex * 2 + f // 8) % 3
                            wdv = WD[ds].rearrange("p (f n) -> p f n", f=8)
                            fns.append(lambda e, f=f, ob=ob, wdv=wdv, s_=s_, hf=hf: e.matmul(
                                banks[ob][:, :], lhsT=HTv[:, f, s_ * 128:(s_ + 1) * 128], rhs=wdv[:, f % 8, hf * 512:(hf + 1) * 512],
                                start=(f == 0), stop=(f == 15)))
                        P.group("pe", fns, reads=[b_HT, b_WD[(ex * 2) % 3], b_WD[(ex * 2 + 1) % 3]], writes=[bankb[ob]])
                        P.op("dve", lambda e, ob=ob, ysl=ysl, hf=hf, s_=s_: e.scalar_tensor_tensor(
                            out=YE[ysl][:, hf * 512:(hf + 1) * 512], in0=banks[ob][:, :], scalar=gslv[:, ex, s_:s_ + 1],
                            in1=g2b[:, hf * 512:(hf + 1) * 512], op0=ALU.mult, op1=ALU.mult),
                            reads=[bankb[ob], b_idxe[ex], b_modb], writes=[b_YE[ysl]])
                    u5 += 1
                    P.dma("pool", lambda e, ysl=ysl, s_=s_: e.indirect_dma_start(
                        out=out_d[:, :], out_offset=bass.IndirectOffsetOnAxis(ap=idxiv[:, ex, s_:s_ + 1], axis=0),
                        in_=YE[ysl], in_offset=None, compute_op=ALU.add), reads=[b_YE[ysl], b_idxe[ex], b_out], writes=[b_out])
                if ex + 1 < NE:
                    load_d(ex + 1, 0)
                    load_d(ex + 1, 1)
            P.barrier()
            ar.reset(m_persist)
            ar.nwords = ar.total
            fnb = ar.alloc(D)
            b_fnb = Buf("fnb")
            P.dma("sp", lambda e: e.dma_start(out=fnb, in_=fnb_d), writes=[b_fnb])
            xo = [ar.alloc(D) for _ in range(4)]
            b_xo = [Buf("xo%d" % i) for i in range(4)]
            yo = [ar.alloc(D) for _ in range(4)]
            b_yo = [Buf("yo%d" % i) for i in range(4)]
            junk = ar.alloc(D)
            b_junk = Buf("junkf")
            sf = ar.alloc(64)
            b_sfl = [Buf("sf%d" % i) for i in range(8)]
            for ti in range(32):
                sl = ti % 4
                b_sf = b_sfl[ti % 8]
                P.dma("sp", lambda e, sl=sl, ti=ti: e.dma_start(out=xo[sl], in_=out_d[ti * 128:(ti + 1) * 128, :]), writes=[b_xo[sl]])
                P.op("act", lambda e, sl=sl, ti=ti: e.activation(out=junk, in_=xo[sl], func=AF.Square, accum_out=sf[:, ti:ti + 1]),
                     reads=[b_xo[sl]], writes=[b_junk, b_sf])
                P.op("act", lambda e, ti=ti: e.activation(out=sf[:, 32 + ti:33 + ti], in_=sf[:, ti:ti + 1], func=AF.Sqrt, bias=epsc[:, 0:1], scale=1.0 / D),
                     reads=[b_sf, b_const], writes=[b_sf])
                P.op("dve", lambda e, ti=ti: e.reciprocal(out=sf[:, 32 + ti:33 + ti], in_=sf[:, 32 + ti:33 + ti]), reads=[b_sf], writes=[b_sf])
                P.op("dve", lambda e, sl=sl, ti=ti: e.scalar_tensor_tensor(out=yo[sl], in0=xo[sl], scalar=sf[:, 32 + ti:33 + ti], in1=fnb,
                                                                        op0=ALU.mult, op1=ALU.mult), reads=[b_xo[sl], b_sf, b_fnb], writes=[b_yo[sl]])
                P.dma("pool", lambda e, sl=sl, ti=ti: e.dma_start(out=out_d[ti * 128:(ti + 1) * 128, :], in_=yo[sl]), reads=[b_yo[sl]], writes=[Buf("outf")])

        P.final_wait("sp")

        with nc.Block() as block:
            @block.tensor
            def _(e):
                P.replay("pe", e)

            @block.scalar
            def _(e):
                P.replay("act", e)

            @block.vector
            def _(e):
                P.replay("dve", e)

            @block.gpsimd
            def _(e):
                P.replay("pool", e)

            @block.sync
            def _(e):
                P.replay("sp", e)
    return nc, list(dbg_d.keys())


def _pp(v, nchunk):
    return np.ascontiguousarray(np.asarray(v, np.float32).reshape(nchunk, 128).T)


def _consts():
    c = {}
    c["ident_f"] = np.eye(128, dtype=np.float32)
    c["ident_b"] = np.eye(128, dtype=np.float32).astype(ml_dtypes.bfloat16)
    t = np.arange(NLAT)
    row = (t // 64).astype(np.float32)
    col = (t % 64).astype(np.float32)
    inv = (10000.0 ** (-np.arange(16, dtype=np.float32) / 16)).astype(np.float32)
    ang_r = row[:, None] * inv[None, :]
    ang_c = col[:, None] * inv[None, :]
    cos_t = np.zeros((128, NLAT), np.float32)
    sin_t = np.zeros((128, NLAT), np.float32)
    rperm = np.zeros((128, 128), np.float32)
    for p in range(128):
        hh, d = divmod(p, 64)
        grp, dd = divmod(d, 32)
        ang = ang_r if grp == 0 else ang_c
        fi = dd % 16
        cos_t[p] = np.cos(ang[:, fi])
        if dd < 16:
            sin_t[p] = -np.sin(ang[:, fi])
            partner = p + 16
        else:
            sin_t[p] = np.sin(ang[:, fi])
            partner = p - 16
        rperm[partner, p] = 1.0
    c["cos_t"] = cos_t
    c["sin_t"] = sin_t
    c["rperm"] = rperm.astype(ml_dtypes.bfloat16)
    c["iota_slot"] = np.broadcast_to(np.arange(CAP, dtype=np.uint16)[None, :], (128, CAP)).copy()
    tok = (np.arange(32)[None, :] * 128 + np.arange(128)[:, None])
    tc = np.stack([tok // 64, tok % 64], axis=-1).astype(np.float32)
    c["tokcols"] = np.broadcast_to(tc[:, :, None, :], (128, 32, 16, 2)).reshape(128, 1024).astype(ml_dtypes.bfloat16)
    pidx = np.arange(128)
    same = (pidx[:, None] // 8) == (pidx[None, :] // 8)
    c["g8"] = same.astype(np.float32)
    c["l8"] = (same & ((pidx[:, None] % 8) < (pidx[None, :] % 8))).astype(np.float32)
    c["sel2"] = np.stack([np.ones(128, np.float32), np.zeros(128, np.float32)])
    c["wcomb"] = np.array([[64, 0], [1, 0], [0, 1], [0, 1], [0, 1]], np.float32)
    kc = np.arange(64)[:, None]
    qc = np.arange(64)[None, :]
    cstart = np.clip(qc - 8, 0, 48)
    ok = ((kc >= cstart) & (kc < cstart + 16)).astype(np.float32)
    ok2 = np.concatenate([ok, ok], axis=0)
    c["maskx"] = np.broadcast_to(ok2[:, None, :], (128, 32, 64)).reshape(128, 2048).copy()
    return c


def _rpb_gather(rpb):
    p = np.arange(128)
    krm = p // 64
    kc = p % 64
    dl = np.arange(8)
    kt = np.arange(4)
    qc = np.arange(64)
    kr = 2 * kt[None, None, :, None] + krm[:, None, None, None]
    dr = np.clip(kr - dl[None, :, None, None] + 7, 0, 14)
    dc = np.clip(kc[:, None, None, None] - qc[None, None, None, :], -15, 15) + 15
    dr = np.broadcast_to(dr, (128, 8, 4, 64))
    dc = np.broadcast_to(dc, (128, 8, 4, 64))
    g = rpb[:, dr, dc]
    return np.ascontiguousarray(g.reshape(16, 128, 2048).astype(np.float32))


def prepare_inputs(inputs):
    f = lambda a: np.ascontiguousarray(np.asarray(a, dtype=np.float32))
    x = f(inputs["x"])
    c = f(inputs["c"])
    ctx = f(inputs["ctx"])
    c_ctx = f(inputs["c_ctx"])
    shared = {}
    shared["w_mod"] = f(inputs["w_mod"][0])
    bm = f(inputs["b_mod"][0])
    shared["bmod_row"] = np.stack([bm, bm])
    shared["bmod_pp"] = _pp(bm, 48)
    shared["w_in"] = f(inputs["w_in"][0])
    bi = f(inputs["b_in"][0])
    shared["bin_pp"] = _pp(bi, 56)
    shared["bin_v"] = np.broadcast_to(bi[4 * D:5 * D][None, :], (128, D)).copy()
    cw = f(inputs["conv_w"][0])
    shared["convw_pp"] = np.ascontiguousarray(cw.T.reshape(8, 128, 4).transpose(1, 0, 2).reshape(128, 32))
    shared["convb_pp"] = _pp(f(inputs["conv_b"][0]), 8)
    wa = f(inputs["lru_wa"][0])
    wi = f(inputs["lru_wi"][0])
    wbd = np.zeros((2, 2, 8, 128, 128), np.float32)
    for d_ in range(2):
        for a_, w in enumerate((wa, wi)):
            for j in range(8):
                wbd[d_, a_, j, 0:64, 0:64] = w[d_, 2 * j]
                wbd[d_, a_, j, 64:128, 64:128] = w[d_, 2 * j + 1]
    shared["lru_wbd"] = wbd
    ba = f(inputs["lru_ba"][0])
    bi_ = f(inputs["lru_bi"][0])
    lb = np.zeros((128, 2, 2, 8), np.float32)
    for d_ in range(2):
        lb[:, d_, 0, :] = _pp(ba[d_], 8)
        lb[:, d_, 1, :] = _pp(bi_[d_], 8)
    shared["lru_b_pp"] = lb.reshape(128, 32)
    lam = f(inputs["lru_lambda"][0])
    shared["lam_pp"] = np.stack([_pp(lam[0], 8), _pp(lam[1], 8)], axis=1).reshape(128, 16)
    shared["rpbg"] = _rpb_gather(f(inputs["na_rpb"][0]))
    shared["w_proj_rnn"] = f(inputs["w_proj_rnn"][0])
    shared["w_proj_na"] = f(inputs["w_proj_na"][0])
    shared["w_out"] = f(inputs["w_out"][0])
    wr = f(inputs["w_router"][0])
    shared["wr_pp"] = np.ascontiguousarray(wr.reshape(8, 128, 16).transpose(1, 0, 2).reshape(128, 128))
    shared["w_exp_gate"] = f(inputs["w_exp_gate"][0])
    shared["w_exp_up"] = f(inputs["w_exp_up"][0])
    shared["w_exp_down"] = f(inputs["w_exp_down"][0])
    shared["fn_b"] = np.broadcast_to(f(inputs["final_norm"])[None, :], (128, D)).copy()
    shared.update(_consts())
    in_maps = []
    for b in range(x.shape[0]):
        m = dict(shared)
        m["x"] = x[b]
        m["ctx"] = ctx[b]
        cpp = np.stack([_pp(c[b], 8), _pp(c_ctx, 8)], axis=-1)
        m["cpp"] = np.ascontiguousarray(cpp.reshape(128, 16))
        in_maps.append(m)
    return in_maps


def kernel(**inputs):
    in_maps = prepare_inputs(inputs)
    nc, _ = build_nc()
    res = run_bass_kernel_spmd(nc, in_maps, core_ids=list(range(len(in_maps))))
    out = np.stack([np.asarray(r["out"], dtype=np.float32) for r in res.results], axis=0)
    return out
```

```python
import numpy as np
from contextlib import ExitStack
import concourse.bass as bass
import concourse.mybir as mybir
from concourse.bass_utils import run_bass_kernel_spmd
import ml_dtypes

F32 = mybir.dt.float32
BF16 = mybir.dt.bfloat16
I32 = mybir.dt.int32
AF = mybir.ActivationFunctionType
ALU = mybir.AluOpType

D = 1024
NLAT = 4096
NCTX = 256
TE = NLAT + NCTX
NE = 16
DE = 2048
CAP = 512
EPS = 1e-6
NPOOL = 88


class Sem:
    def __init__(self, h, name):
        self.h = h
        self.name = name
        self.n = 0


class Buf:
    def __init__(self, name):
        self.name = name
        self.w = {}
        self.r = {}


class _Rec:
    def __init__(self):
        self.call = None

    def __getattr__(self, name):
        def f(*a, **k):
            self.call = (name, a, k)
        return f


def _capture(fn):
    r = _Rec()
    fn(r)
    assert r.call is not None
    return r.call


class Prog:
    ENG = ["pe", "act", "dve", "pool", "sp"]

    def __init__(self, nc, es):
        self.nc = nc
        self.es = es
        self.q = {e: [] for e in self.ENG}
        self.esem = {e: self.newsem("e_" + e) for e in ["pe", "act", "dve", "pool"]}
        self.waited = {e: {} for e in self.ENG}
        self.dpools = {"sp": [self.newsem("ds%d" % i) for i in range(NPOOL // 2)],
                       "pool": [self.newsem("dp%d" % i) for i in range(NPOOL // 2)]}
        self.dpool = self.dpools["sp"] + self.dpools["pool"]
        self.di = {"sp": 0, "pool": 0}
        self.floor = {}

    def newsem(self, name):
        h = self.es.enter_context(self.nc.semaphore(name))
        return Sem(h, name)

    def _deps(self, eng, reads, writes):
        deps = dict(self.floor)

        def add(d):
            for k, (s, v) in d.items():
                if k not in deps or deps[k][1] < v:
                    deps[k] = (s, v)

        for b in reads:
            add(b.w)
        for b in writes:
            add(b.w)
            add(b.r)
        waits = []
        own = self.esem.get(eng)
        for k, (s, v) in deps.items():
            if s is own and eng == "pe":
                continue
            if self.waited[eng].get(k, 0) >= v:
                continue
            self.waited[eng][k] = v
            waits.append((s, v))
        return waits

    def _mark(self, tok, reads, writes):
        s, v = tok
        for b in reads:
            b.r[id(s)] = (s, v)
        for b in writes:
            b.w[id(s)] = (s, v)

    def op(self, eng, fn, reads=(), writes=()):
        self.group(eng, [fn], reads, writes)

    def group(self, eng, fns, reads=(), writes=()):
        waits = self._deps(eng, reads, writes)
        s = self.esem[eng]
        s.n += 1
        n = len(fns)
        for i, fn in enumerate(fns):
            self.q[eng].append((waits if i == 0 else (), _capture(fn), (s, 1) if i == n - 1 else None))
        self._mark((s, s.n), reads, writes)

    def dma(self, qeng, fn, reads=(), writes=()):
        waits = self._deps(qeng, reads, writes)
        pl = self.dpools[qeng]
        s = pl[self.di[qeng] % len(pl)]
        self.di[qeng] += 1
        if s.n > 0 and self.waited[qeng].get(id(s), 0) < s.n:
            waits.append((s, s.n))
            self.waited[qeng][id(s)] = s.n
        s.n += 16
        self.q[qeng].append((waits, _capture(fn), (s, 16)))
        self._mark((s, s.n), reads, writes)

    def barrier(self):
        for s in list(self.esem.values()) + self.dpool:
            if s.n > 0:
                self.floor[id(s)] = (s, s.n)

    def final_wait(self, eng):
        self.barrier()
        waits = self._deps(eng, (), ())
        self.q[eng].append((waits, None, None))

    def replay(self, eng, e):
        for waits, fn, inc in self.q[eng]:
            for (s, v) in waits:
                e.wait_ge(s.h, v)
            if fn is None:
                continue
            name, a, k = fn
            ins = getattr(e, name)(*a, **k)
            if inc is not None:
                ins.then_inc(inc[0].h, inc[1])


class Arena:
    def __init__(self, ap_f32, nwords):
        self.base = ap_f32
        self.nwords = nwords
        self.total = nwords
        self.off = 0

    def alloc_top(self, nelem, dtype=F32):
        nw = nelem if dtype in (F32, I32) else (nelem + 1) // 2
        nw = (nw + 1) // 2 * 2
        self.nwords -= nw
        assert self.nwords >= self.off
        v = self.base[:, self.nwords:self.nwords + nw]
        return v[:, 0:nelem] if dtype == F32 else v.bitcast(dtype)[:, 0:nelem]

    def mark(self):
        return self.off

    def reset(self, m):
        self.off = m

    def alloc(self, nelem, dtype=F32):
        if dtype == F32 or dtype == I32:
            nw = nelem
        else:
            nw = (nelem + 1) // 2
        nw = (nw + 1) // 2 * 2
        assert self.off + nw <= self.nwords, ("SBUF arena overflow", self.off, nw, self.nwords)
        v = self.base[:, self.off:self.off + nw]
        self.off += nw
        if dtype == F32:
            return v[:, 0:nelem]
        return v.bitcast(dtype)[:, 0:nelem]


def ntiles(n0, n1, step=512):
    out = []
    t = n0
    while t < n1:
        out.append((t, min(t + step, n1)))
        t += step
    return out


def build_nc(stage=99, dbg=False, na_pairs=8, na_rows=64, skip_lru=False):
    nc = bass.Bass("TRN2", target_bir_lowering=False)

    def din(name, shape, dt=F32):
        return nc.dram_tensor(name, list(shape), dt, kind="ExternalInput").ap()

    x_d = din("x", [NLAT, D])
    ctx_d = din("ctx", [NCTX, D])
    cpp_d = din("cpp", [128, 16])
    wmod_d = din("w_mod", [D, 6 * D])
    bmodrow_d = din("bmod_row", [2, 6 * D])
    bmodpp_d = din("bmod_pp", [128, 48])
    win_d = din("w_in", [D, 7 * D])
    binpp_d = din("bin_pp", [128, 56])
    binv_d = din("bin_v", [128, D])
    convw_d = din("convw_pp", [128, 32])
    convb_d = din("convb_pp", [128, 8])
    lruw_d = din("lru_wbd", [2, 2, 8, 128, 128])
    lrub_d = din("lru_b_pp", [128, 32])
    lam_d = din("lam_pp", [128, 16])
    rpbg_d = din("rpbg", [16, 128, 2048])
    maskx_d = din("maskx", [128, 2048])
    wpr_d = din("w_proj_rnn", [D, D])
    wpn_d = din("w_proj_na", [D, D])
    wout_d = din("w_out", [D, D])
    wrt_d = din("wr_pp", [128, 128])
    wg_d = din("w_exp_gate", [NE, D, DE])
    wu_d = din("w_exp_up", [NE, D, DE])
    wd_d = din("w_exp_down", [NE, DE, D])
    fnb_d = din("fn_b", [128, D])
    identf_d = din("ident_f", [128, 128])
    identb_d = din("ident_b", [128, 128], BF16)
    rperm_d = din("rperm", [128, 128], BF16)
    cos_d = din("cos_t", [128, NLAT])
    sin_d = din("sin_t", [128, NLAT])
    iota_d = din("iota_slot", [128, CAP], mybir.dt.uint16)
    tokc_d = din("tokcols", [128, 1024], BF16)
    sel_d = din("sel2", [2, 128])
    g8_d = din("g8", [128, 128])
    l8_d = din("l8", [128, 128])
    wc_d = din("wcomb", [5, 2])
    out_d = nc.dram_tensor("out", [NLAT, D], F32, kind="ExternalOutput").ap()
    yrT_d = nc.dram_tensor("yrT_s", [8, 128, 8, 512], BF16, kind="Internal").ap()
    ynT_d = nc.dram_tensor("ynT_s", [8, 128, 8, 512], BF16, kind="Internal").ap()
    xn2_d = nc.dram_tensor("xn2_s", [NLAT, D], BF16, kind="Internal").ap()
    modb_d = nc.dram_tensor("modb_s", [128, 4096], F32, kind="Internal").ap()
    sg_d = nc.dram_tensor("sg_s", [2, 8, 128, 8, 512], BF16, kind="Internal").ap()
    dbg_d = {}

    def dout(name, shape, dt=F32):
        dbg_d[name] = nc.dram_tensor(name, list(shape), dt, kind="ExternalOutput").ap()
        return dbg_d[name]

    es = ExitStack()
    with es:
        NW = 52800
        arena_t = es.enter_context(nc.sbuf_tensor("arena", [128, NW], F32))
        ar = Arena(arena_t[:, :], NW)
        banks = []
        for i in range(8):
            pt = es.enter_context(nc.psum_tensor("bank%d" % i, [128, 512], F32))
            banks.append(pt[:, :])
        bankb = [Buf("bank%d" % i) for i in range(8)]
        P = Prog(nc, es)

        b_yrT = Buf("yrT")
        b_ynT = Buf("ynT")
        b_xn2d = Buf("xn2d")
        b_out = Buf("out")

        identf = ar.alloc(128)
        identb = ar.alloc(128, BF16)
        mp = ar.alloc(32)
        epsc = ar.alloc(2)
        b_const = Buf("const")
        b_mp = Buf("mp")
        b_modb = Buf("modb")
        P.dma("sp", lambda e: e.dma_start(out=identf, in_=identf_d), writes=[b_const])
        P.dma("sp", lambda e: e.dma_start(out=identb, in_=identb_d), writes=[b_const])
        P.op("dve", lambda e: e.memset(epsc[:, 0:1], EPS), writes=[b_const])
        P.op("dve", lambda e: e.memset(epsc[:, 1:2], 1.0), writes=[b_const])
        m_persist = ar.mark()
        modb = ar.alloc(4096)
        b_modbd = Buf("modb_d")

        cpp = ar.alloc(16)
        s2 = ar.alloc(16, BF16)
        bmpp = ar.alloc(48)
        modrow = ar.alloc(4096)
        bmrow = ar.alloc(4096)
        sel = ar.alloc(128)
        wm = [ar.alloc(8 * 512, BF16) for _ in range(4)]
        b_cpp, b_s2, b_bm, b_modrow, b_sel = Buf("cpp"), Buf("s2"), Buf("bm"), Buf("modrow"), Buf("sel")
        b_wm = [Buf("wm%d" % i) for i in range(4)]
        P.dma("sp", lambda e: e.dma_start(out=cpp, in_=cpp_d), writes=[b_cpp])
        P.dma("sp", lambda e: e.dma_start(out=bmpp, in_=bmodpp_d), writes=[b_bm])
        P.dma("sp", lambda e: e.dma_start(out=bmrow[0:2, :], in_=bmodrow_d[:, 2048:6144]), writes=[b_bm])
        P.dma("sp", lambda e: e.dma_start(out=sel[0:2, :], in_=sel_d), writes=[b_sel])
        P.op("act", lambda e: e.activation(out=s2, in_=cpp, func=AF.Silu), reads=[b_cpp], writes=[b_s2])
        s2v = s2.rearrange("p (k s) -> p k s", s=2)
        wmod_v = wmod_d.rearrange("(k p) j -> p k j", p=128)
        for i in range(12):
            sl = i % 4
            wmv = wm[sl].rearrange("p (k j) -> p k j", k=8)
            P.dma("pool", lambda e, wmv=wmv, i=i: e.dma_start(out=wmv, in_=wmod_v[:, :, i * 512:(i + 1) * 512]),
                  writes=[b_wm[sl]])
            if i < 4:
                for jc in range(4):
                    j = 4 * i + jc
                    fns = []
                    for k in range(8):
                        fns.append(lambda e, wmv=wmv, k=k, jc=jc, j=j: e.matmul(
                            banks[0][:, 2 * j:2 * j + 2], lhsT=wmv[:, k, jc * 128:(jc + 1) * 128], rhs=s2v[:, k, :],
                            start=(k == 0), stop=(k == 7)))
                    P.group("pe", fns, reads=[b_wm[sl], b_s2], writes=[bankb[0]])
            else:
                bk = 1 + (i % 2)
                fns = []
                for k in range(8):
                    fns.append(lambda e, wmv=wmv, k=k, bk=bk: e.matmul(
                        banks[bk][0:2, :], lhsT=s2v[:, k, :], rhs=wmv[:, k, :], start=(k == 0), stop=(k == 7)))
                P.group("pe", fns, reads=[b_wm[sl], b_s2], writes=[bankb[bk]])
                o = (i - 4) * 512
                P.op("dve", lambda e, bk=bk, o=o: e.tensor_tensor(
                    out=modrow[0:2, o:o + 512], in0=banks[bk][0:2, :], in1=bmrow[0:2, o:o + 512], op=ALU.add),
                    reads=[bankb[bk], b_bm], writes=[b_modrow])
        mpv = mp.rearrange("p (s j) -> p s j", s=2)
        b0v = banks[0][:, 0:32].rearrange("p (j s) -> p j s", s=2)
        for s_ in range(2):
            P.op("dve", lambda e, s_=s_: e.tensor_tensor(out=mpv[:, s_, :], in0=b0v[:, :, s_], in1=bmpp[:, 0:16], op=ALU.add),
                 reads=[bankb[0], b_bm], writes=[b_mp])
            P.op("dve", lambda e, s_=s_: e.tensor_scalar(out=mpv[:, s_, 8:16], in0=mpv[:, s_, 8:16], scalar1=1.0, scalar2=None,
                                                       op0=ALU.add), reads=[b_mp], writes=[b_mp])
        for n in range(8):
            bk = 3 + (n % 2)
            P.op("pe", lambda e, bk=bk, n=n: e.matmul(banks[bk][:, :], lhsT=sel[0:2, :], rhs=modrow[0:2, n * 512:(n + 1) * 512],
                                                    start=True, stop=True), reads=[b_sel, b_modrow], writes=[bankb[bk]])
            if n in (4, 5):
                P.op("dve", lambda e, bk=bk, n=n: e.tensor_scalar(out=modb[:, n * 512:(n + 1) * 512], in0=banks[bk][:, :],
                                                                scalar1=1.0, scalar2=None, op0=ALU.add),
                     reads=[bankb[bk]], writes=[b_modb])
            else:
                P.op("act", lambda e, bk=bk, n=n: e.activation(out=modb[:, n * 512:(n + 1) * 512], in_=banks[bk][:, :],
                                                             func=AF.Identity), reads=[bankb[bk]], writes=[b_modb])
        P.dma("sp", lambda e: e.dma_start(out=modb_d, in_=modb), reads=[b_modb], writes=[b_modbd])
        if dbg:
            d_mp = dout("d_mp", [128, 32])
            d_modb = dout("d_modb", [128, 4096])
            P.dma("sp", lambda e: e.dma_start(out=d_mp, in_=mp), reads=[b_mp])
            P.dma("sp", lambda e: e.dma_start(out=d_modb, in_=modb), reads=[b_modb])
        P.barrier()
        ar.reset(m_persist)

        xnT = ar.alloc(8 * TE, BF16)
        xnTv = xnT.rearrange("p (k t) -> p k t", k=8)
        b_xnT = [Buf("xnT%d" % i) for i in range(34)]
        m_xnT = ar.mark()

        def xn_bufs(t0, t1):
            return [b_xnT[i] for i in range(t0 // 128, (t1 + 127) // 128)]

        if stage >= 1:
            xin = [ar.alloc(1024) for _ in range(4)]
            xnf = [ar.alloc(1024) for _ in range(4)]
            junk = ar.alloc(1024)
            ss = ar.alloc(34)
            rstd = ar.alloc(34)
            b_xin = [Buf("xin%d" % i) for i in range(4)]
            b_xnf = [Buf("xnf%d" % i) for i in range(4)]
            b_junk = Buf("junk")
            b_ssl = [Buf("ss%d" % i) for i in range(8)]
            def p1_A(ti):
                sl = ti % 4
                b_ss = b_ssl[ti % 8]
                src = ctx_d[ti * 128:(ti + 1) * 128, :] if ti < 2 else x_d[(ti - 2) * 128:(ti - 1) * 128, :]
                P.dma("sp", lambda e: e.dma_start(out=xin[sl], in_=src), writes=[b_xin[sl]])
                P.op("act", lambda e: e.activation(out=junk, in_=xin[sl], func=AF.Square, accum_out=ss[:, ti:ti + 1]),
                     reads=[b_xin[sl]], writes=[b_junk, b_ss])
                P.op("act", lambda e: e.activation(out=rstd[:, ti:ti + 1], in_=ss[:, ti:ti + 1], func=AF.Sqrt,
                                                   bias=epsc[:, 0:1], scale=1.0 / D), reads=[b_ss, b_const], writes=[b_ss])
                P.op("dve", lambda e: e.reciprocal(out=rstd[:, ti:ti + 1], in_=rstd[:, ti:ti + 1]), reads=[b_ss], writes=[b_ss])
                P.op("dve", lambda e: e.tensor_scalar(out=xnf[sl], in0=xin[sl], scalar1=rstd[:, ti:ti + 1], scalar2=None,
                                                     op0=ALU.mult), reads=[b_xin[sl], b_ss], writes=[b_xnf[sl]])

            def p1_B(ti):
                sl = ti % 4
                s_ = 1 if ti < 2 else 0
                bA = 2 * (ti % 4)
                for hb in range(2):
                    bk = bA + hb
                    fns = []
                    for kk in range(4):
                        k = hb * 4 + kk
                        fns.append(lambda e, bk=bk, kk=kk, k=k: e.transpose(
                            banks[bk][:, kk * 128:(kk + 1) * 128], xnf[sl][:, k * 128:(k + 1) * 128], identf))
                    P.group("pe", fns, reads=[b_xnf[sl], b_const], writes=[bankb[bk]])
                    for kk in range(4):
                        k = hb * 4 + kk
                        dst = xnTv[:, k, ti * 128:(ti + 1) * 128]
                        if k % 2 == 0:
                            P.op("act", lambda e, bk=bk, kk=kk, k=k, dst=dst: e.activation(
                                out=dst, in_=banks[bk][:, kk * 128:(kk + 1) * 128], func=AF.Identity,
                                bias=mpv[:, s_, k:k + 1], scale=mpv[:, s_, 8 + k:9 + k]),
                                reads=[bankb[bk], b_mp], writes=[b_xnT[ti]])
                        else:
                            P.op("dve", lambda e, bk=bk, kk=kk, k=k, dst=dst: e.tensor_scalar(
                                out=dst, in0=banks[bk][:, kk * 128:(kk + 1) * 128], scalar1=mpv[:, s_, 8 + k:9 + k],
                                scalar2=mpv[:, s_, k:k + 1], op0=ALU.mult, op1=ALU.add),
                                reads=[bankb[bk], b_mp], writes=[b_xnT[ti]])

            p1_A(0)
            p1_A(1)
            for ti in range(34):
                if ti + 2 < 34:
                    p1_A(ti + 2)
                p1_B(ti)
            if dbg:
                d_xnT = dout("d_xnT", [128, 8 * TE], BF16)
                P.dma("sp", lambda e: e.dma_start(out=d_xnT, in_=xnT), reads=b_xnT)
            P.barrier()
            ar.reset(m_xnT)

        win_v = win_d.rearrange("(k p) n -> p k n", p=128)
        b_sgd = Buf("sg_d")

        if stage >= 2 and not skip_lru:
            m0 = ar.mark()
            cw = ar.alloc(32)
            cb = ar.alloc(8)
            lb = ar.alloc(32)
            lam = ar.alloc(16)
            cneg = ar.alloc(16)
            cneg2 = ar.alloc(16)
            binpp = ar.alloc(56)
            b_lc = Buf("lru_consts")
            for (dst, src) in ((cw, convw_d), (cb, convb_d), (lb, lrub_d), (lam, lam_d), (binpp, binpp_d)):
                P.dma("sp", lambda e, dst=dst, src=src: e.dma_start(out=dst, in_=src), writes=[b_lc])
            P.op("act", lambda e: e.activation(out=cneg, in_=lam, func=AF.Exp, scale=-1.0), reads=[b_lc], writes=[b_lc])
            P.op("act", lambda e: e.activation(out=cneg, in_=cneg, func=AF.Ln, bias=epsc[:, 1:2], scale=1.0), reads=[b_lc, b_const], writes=[b_lc])
            P.op("dve", lambda e: e.tensor_scalar(out=cneg2, in0=cneg, scalar1=-16.0, scalar2=None, op0=ALU.mult), reads=[b_lc], writes=[b_lc])
            P.op("dve", lambda e: e.tensor_scalar(out=cneg, in0=cneg, scalar1=-8.0, scalar2=None, op0=ALU.mult), reads=[b_lc], writes=[b_lc])
            cwv = cw.rearrange("p (j k) -> p j k", k=4)
            lbv = lb.rearrange("p (d a j) -> p d a j", d=2, a=2)
            cnv = cneg.rearrange("p (d j) -> p d j", d=2)
            cn2v = cneg2.rearrange("p (d j) -> p d j", d=2)
            _wl = ar.alloc(8 * 256, BF16)
            wl = [_wl, _wl]
            wgc2 = [ar.alloc(8 * 128, BF16), ar.alloc(8 * 128, BF16)]
            b_wgc2 = [Buf("wgc2a"), Buf("wgc2b")]
            wg4 = [ar.alloc(4 * 128, BF16), ar.alloc(4 * 128, BF16)]
            _bwl = Buf("wl")
            b_wl = [_bwl, _bwl]
            b_wg4 = [Buf("wg40"), Buf("wg41")]
            XR = ar.alloc(TE)
            XC = ar.alloc(TE)
            AA = ar.alloc(TE)
            I0 = ar.alloc(TE)
            I1 = ar.alloc(TE)
            XCB = ar.alloc(TE, BF16)
            GYs = [ar.alloc(NLAT, BF16), ar.alloc(NLAT, BF16)]
            b_GYs = [Buf("GY0"), Buf("GY1")]
            XIN = ar.alloc(TE, BF16)
            b_XIN = Buf("XIN")
            _yb = ar.alloc(NLAT, BF16)
            YB = [_yb, _yb]
            b_XR, b_XC, b_AA, b_I0, b_I1, b_XCB = (Buf(n) for n in ("XR", "XC", "AA", "I0", "I1", "XCB"))
            _byb = Buf("YB")
            b_YB = [_byb, _byb]
            IB = [I0, I1]
            b_IB = [b_I0, b_I1]
            etiles = ntiles(0, TE)
            ltiles = ntiles(NCTX, TE)
            lbank = [0]

            def nextbank():
                lbank[0] = (lbank[0] + 1) % 4
                return lbank[0]

            def load_lru_w(j):
                sl = j % 2
                wlv = wl[sl].rearrange("p (k n) -> p k n", k=8)
                P.dma("pool", lambda e: e.dma_start(out=wlv[:, :, 0:128], in_=win_v[:, :, j * 128:(j + 1) * 128]), writes=[b_wl[sl]])
                P.dma("pool", lambda e: e.dma_start(out=wlv[:, :, 128:256], in_=win_v[:, :, D + j * 128:D + (j + 1) * 128]), writes=[b_wl[sl]])
                w4v = wg4[sl].rearrange("p (g m) -> p g m", g=4)
                for d_ in range(2):
                    for a_ in range(2):
                        P.dma("pool", lambda e, d_=d_, a_=a_: e.dma_start(out=w4v[:, d_ * 2 + a_, :], in_=lruw_d[d_, a_, j]), writes=[b_wg4[sl]])

            def in_proj(j):
                sl = j % 2
                wlv = wl[sl].rearrange("p (k n) -> p k n", k=8)
                for (t0, t1) in etiles:
                    bk = nextbank()
                    fns = [(lambda e, k=k, bk=bk, t0=t0, t1=t1: e.matmul(banks[bk][:, 0:t1 - t0], lhsT=wlv[:, k, 128:256], rhs=xnTv[:, k, t0:t1],
                                                                      start=(k == 0), stop=(k == 7))) for k in range(8)]
                    P.group("pe", fns, reads=[b_wl[sl]] + xn_bufs(t0, t1), writes=[bankb[bk]])
                    P.op("act", lambda e, bk=bk, t0=t0, t1=t1: e.activation(out=XIN[:, t0:t1], in_=banks[bk][:, 0:t1 - t0], func=AF.Identity,
                                                                          bias=binpp[:, 8 + j:9 + j], scale=1.0),
                         reads=[bankb[bk], b_lc], writes=[b_XIN])
                for (t0, t1) in ltiles:
                    bk = nextbank()
                    fns = [(lambda e, k=k, bk=bk, t0=t0, t1=t1: e.matmul(banks[bk][:, 0:t1 - t0], lhsT=wlv[:, k, 0:128], rhs=xnTv[:, k, t0:t1],
                                                                      start=(k == 0), stop=(k == 7))) for k in range(8)]
                    P.group("pe", fns, reads=[b_wl[sl]] + xn_bufs(t0, t1), writes=[bankb[bk]])
                    P.op("act", lambda e, bk=bk, t0=t0, t1=t1: e.activation(out=GYs[sl][:, t0 - NCTX:t1 - NCTX], in_=banks[bk][:, 0:t1 - t0],
                                                                          func=AF.Gelu_apprx_tanh, bias=binpp[:, j:j + 1], scale=1.0),
                         reads=[bankb[bk], b_lc], writes=[b_GYs[sl]])

            def gates(j, d_, a_):
                sl = j % 2
                w4v = wg4[sl].rearrange("p (g m) -> p g m", g=4)
                dst = XR if a_ == 0 else IB[d_]
                bdst = b_XR if a_ == 0 else b_IB[d_]
                for (t0, t1) in etiles:
                    bk = nextbank()
                    P.op("pe", lambda e, bk=bk, t0=t0, t1=t1: e.matmul(
                        banks[bk][:, 0:t1 - t0], lhsT=w4v[:, d_ * 2 + a_, :], rhs=XCB[:, t0:t1], start=True, stop=True),
                        reads=[b_wg4[sl], b_XCB], writes=[bankb[bk]])
                    P.op("act", lambda e, bk=bk, t0=t0, t1=t1: e.activation(
                        out=dst[:, t0:t1], in_=banks[bk][:, 0:t1 - t0], func=AF.Sigmoid,
                        bias=lbv[:, d_, a_, j:j + 1], scale=1.0), reads=[bankb[bk], b_lc], writes=[bdst])

            def exps(j, d_):
                P.op("act", lambda e: e.activation(out=AA, in_=XR, func=AF.Exp, scale=cnv[:, d_, j:j + 1]), reads=[b_XR, b_lc], writes=[b_AA])
                P.op("act", lambda e: e.activation(out=XR, in_=XR, func=AF.Exp, scale=cn2v[:, d_, j:j + 1]), reads=[b_XR, b_lc], writes=[b_XR])
                P.op("act", lambda e: e.activation(out=XR, in_=XR, func=AF.Sqrt, bias=epsc[:, 1:2], scale=-1.0), reads=[b_XR, b_const], writes=[b_XR])

            def mulxc(d_):
                Ib, bI = IB[d_], b_IB[d_]
                P.op("dve", lambda e: e.tensor_tensor(out=Ib, in0=Ib, in1=XC, op=ALU.mult), reads=[bI, b_XC], writes=[bI])

            def scan(d_):
                Ib, bI = IB[d_], b_IB[d_]
                P.op("dve", lambda e: e.tensor_tensor(out=Ib, in0=Ib, in1=XR, op=ALU.mult), reads=[bI, b_XR], writes=[bI])
                if d_ == 0:
                    P.op("dve", lambda e: e.tensor_tensor_scan(out=Ib, data0=AA, data1=Ib, initial=0.0, op0=ALU.mult, op1=ALU.add),
                         reads=[bI, b_AA], writes=[bI])
                else:
                    P.op("dve", lambda e: e.tensor_tensor_scan(out=Ib[:, 0:NCTX][:, ::-1], data0=AA[:, 0:NCTX][:, ::-1],
                                                               data1=Ib[:, 0:NCTX][:, ::-1], initial=0.0,
                                                               op0=ALU.mult, op1=ALU.add), reads=[bI, b_AA], writes=[bI])
                    P.op("dve", lambda e: e.tensor_tensor_scan(out=Ib[:, NCTX:TE][:, ::-1], data0=AA[:, NCTX:TE][:, ::-1],
                                                               data1=Ib[:, NCTX:TE][:, ::-1], initial=Ib[:, 0:1],
                                                               op0=ALU.mult, op1=ALU.add), reads=[bI, b_AA], writes=[bI])

            def conv(j):
                P.op("dve", lambda e: e.tensor_scalar(out=XC, in0=XIN, scalar1=cwv[:, j, 2:3], scalar2=cb[:, j:j + 1], op0=ALU.mult, op1=ALU.add),
                     reads=[b_XIN, b_lc], writes=[b_XC])
                for kk in (0, 1, 3):
                    o = kk - 2
                    for (s0, s1) in ((0, NCTX), (NCTX, TE)):
                        a_ = max(s0, s0 - o)
                        b_ = min(s1, s1 - o)
                        P.op("dve", lambda e, a_=a_, b_=b_, o=o, kk=kk: e.scalar_tensor_tensor(
                            out=XC[:, a_:b_], in0=XIN[:, a_ + o:b_ + o], scalar=cwv[:, j, kk:kk + 1], in1=XC[:, a_:b_],
                            op0=ALU.mult, op1=ALU.add), reads=[b_XIN, b_XC, b_lc], writes=[b_XC])
                P.op("act", lambda e: e.activation(out=XCB, in_=XC, func=AF.Identity), reads=[b_XC], writes=[b_XCB])

            def load_gc_l(u):
                wv = wgc2[u % 2].rearrange("p (k n) -> p k n", k=8)
                P.dma("pool", lambda e: e.dma_start(out=wv, in_=win_v[:, :, 5 * D + u * 128:5 * D + (u + 1) * 128]), writes=[b_wgc2[u % 2]])

            def gate_unit(u):
                wv = wgc2[u % 2].rearrange("p (k n) -> p k n", k=8)
                for (t0, t1) in ltiles:
                    bk = nextbank()
                    fns = [(lambda e, k=k, bk=bk, t0=t0, t1=t1: e.matmul(banks[bk][:, 0:t1 - t0], lhsT=wv[:, k, :], rhs=xnTv[:, k, t0:t1],
                                                                      start=(k == 0), stop=(k == 7))) for k in range(8)]
                    P.group("pe", fns, reads=[b_wgc2[u % 2]] + xn_bufs(t0, t1), writes=[bankb[bk]])
                    P.op("act", lambda e, bk=bk, t0=t0, t1=t1: e.activation(out=YB[0][:, t0 - NCTX:t1 - NCTX], in_=banks[bk][:, 0:t1 - t0], func=AF.Sigmoid,
                                                                          bias=binpp[:, 40 + u:41 + u], scale=1.0), reads=[bankb[bk], b_lc], writes=[b_YB[0]])
                P.dma("sp", lambda e: e.dma_start(out=sg_d[u // 8, :, :, u % 8, :].rearrange("t p c -> p t c"), in_=YB[0].rearrange("p (t c) -> p t c", t=8)),
                      reads=[b_YB[0]], writes=[b_sgd])

            load_gc_l(0)
            load_lru_w(0)
            in_proj(0)
            conv(0)
            gates(0, 0, 0)
            gates(0, 0, 1)
            gates(0, 1, 1)
            for j in range(8):
                sl = j % 2
                nxt = j + 1 < 8
                if nxt:
                    load_lru_w(j + 1)
                exps(j, 0)
                mulxc(0)
                mulxc(1)
                scan(0)
                if nxt:
                    in_proj(j + 1)
                gates(j, 1, 0)
                exps(j, 1)
                if nxt:
                    load_gc_l(j + 1)
                gate_unit(j)
                if nxt:
                    conv(j + 1)
                scan(1)
                P.op("pool", lambda e: e.tensor_tensor(out=I1[:, NCTX:TE], in0=I0[:, NCTX:TE], in1=I1[:, NCTX:TE], op=ALU.add),
                     reads=[b_I0, b_I1], writes=[b_I1])
                if nxt:
                    gates(j + 1, 0, 0)
                    gates(j + 1, 0, 1)
                P.op("dve", lambda e, sl=sl: e.tensor_tensor(out=YB[sl], in0=I1[:, NCTX:TE], in1=GYs[sl], op=ALU.mult),
                     reads=[b_I1, b_GYs[sl]], writes=[b_YB[sl]])
                P.dma("sp", lambda e, sl=sl: e.dma_start(out=yrT_d[:, :, j, :].rearrange("t p c -> p t c"), in_=YB[sl].rearrange("p (t c) -> p t c", t=8)),
                      reads=[b_YB[sl]], writes=[b_yrT])
                if nxt:
                    gates(j + 1, 1, 1)
            P.barrier()
            ar.reset(m0)

        if stage >= 3:
            m0 = ar.mark()
            cosT = ar.alloc(NLAT)
            sinT = ar.alloc(NLAT)
            rperm = ar.alloc(128, BF16)
            maskx = ar.alloc(2048)
            binpp = ar.alloc(56)
            b_nc = Buf("na_consts")
            for (dst, src) in ((cosT, cos_d), (sinT, sin_d), (rperm, rperm_d), (maskx, maskx_d), (binpp, binpp_d)):
                P.dma("sp", lambda e, dst=dst, src=src: e.dma_start(out=dst, in_=src), writes=[b_nc])
            wq = [ar.alloc(8 * 384, BF16), ar.alloc(8 * 384, BF16)]
            b_wq = [Buf("wq0"), Buf("wq1")]
            rpf = ar.alloc(2048)
            b_rpf = Buf("rpf")
            expb = ar.alloc(4096, BF16)
            expbv = expb.rearrange("p (d h c) -> p d h c", d=8, h=2)
            _bexp = Buf("expb")
            b_expb = [_bexp, _bexp]
            VTs = [ar.alloc(512, BF16), ar.alloc(512, BF16)]
            b_VTs = [Buf("VT0"), Buf("VT1")]
            QFs = [ar.alloc(512, BF16), ar.alloc(512, BF16)]
            b_QFs = [Buf("QF0"), Buf("QF1")]
            T1s = [ar.alloc(512), ar.alloc(512)]
            T2s = [ar.alloc(512), ar.alloc(512)]
            b_T1s = [Buf("T10"), Buf("T11")]
            b_T2s = [Buf("T20"), Buf("T21")]
            QR = ar.alloc(NLAT, BF16)
            KR = ar.alloc(TE, BF16)
            b_QR, b_KR = Buf("QR"), Buf("KR")
            VE = ar.alloc(34 * 130, BF16)
            VO = ar.alloc(31 * 130, BF16)
            b_VE, b_VO = Buf("VE"), Buf("VO")
            VEv = VE.rearrange("p (t h c) -> p t h c", t=34, h=2)
            VOv = VO.rearrange("p (t h c) -> p t h c", t=31, h=2)
            EB = [ar.alloc(768, BF16) for _ in range(3)]
            b_EB = [Buf("EB%d" % i) for i in range(3)]
            YT = [ar.alloc(128), ar.alloc(128)]
            b_YT = [Buf("YT0"), Buf("YT1")]
            RD = ar.alloc(16)
            b_RD = Buf("RD")
            YN = [ar.alloc(NLAT, BF16), ar.alloc(NLAT, BF16)]
            b_YN = [Buf("YN0"), Buf("YN1")]
            P.op("pool", lambda e: e.memset(VE, 1.0), writes=[b_VE])
            P.op("pool", lambda e: e.memset(VO, 1.0), writes=[b_VO])

            def load_na_w(hp):
                sl = hp % 2
                wv_ = wq[sl].rearrange("p (k n) -> p k n", k=8)
                for i_, base in enumerate((2 * D, 3 * D, 4 * D)):
                    P.dma("pool", lambda e, i_=i_, base=base: e.dma_start(out=wv_[:, :, i_ * 128:(i_ + 1) * 128],
                                                                       in_=win_v[:, :, base + hp * 128:base + (hp + 1) * 128]),
                          writes=[b_wq[sl]])

            load_na_w(0)
            ucount = 0
            for hp in range(na_pairs):
                sl = hp % 2
                if hp + 1 < na_pairs:
                    load_na_w(hp + 1)
                wv_ = wq[sl].rearrange("p (k n) -> p k n", k=8)
                qk_tiles = [(0, t0, t1) for (t0, t1) in ntiles(NCTX, TE)] + [(1, t0, t1) for (t0, t1) in [(0, NCTX)] + ntiles(NCTX, TE)]

                def qk_proj(idx):
                    which, t0, t1 = qk_tiles[idx]
                    bk = idx % 2
                    fns = [(lambda e, k=k: e.matmul(banks[bk][:, 0:t1 - t0], lhsT=wv_[:, k, which * 128:(which + 1) * 128], rhs=xnTv[:, k, t0:t1],
                                                    start=(k == 0), stop=(k == 7))) for k in range(8)]
                    P.group("pe", fns, reads=[b_wq[sl]] + xn_bufs(t0, t1), writes=[bankb[bk]])

                def qk_rope(idx):
                    which, t0, t1 = qk_tiles[idx]
                    n = t1 - t0
                    bk = idx % 2
                    bcol = (2 + which) * 8 + hp
                    if which == 1 and t1 <= NCTX:
                        P.op("act", lambda e: e.activation(out=KR[:, t0:t1], in_=banks[bk][:, 0:n], func=AF.Identity, bias=binpp[:, bcol:bcol + 1], scale=1.0),
                             reads=[bankb[bk], b_nc], writes=[b_KR])
                        return
                    qs = idx % 2
                    qf, t1b, t2b = QFs[qs], T1s[qs], T2s[qs]
                    P.op("act", lambda e: e.activation(out=qf[:, 0:n], in_=banks[bk][:, 0:n], func=AF.Identity, bias=binpp[:, bcol:bcol + 1], scale=1.0),
                         reads=[bankb[bk], b_nc], writes=[b_QFs[qs]])
                    bk2 = 2 + (idx % 2)
                    P.op("pe", lambda e: e.matmul(banks[bk2][:, 0:n], lhsT=rperm, rhs=qf[:, 0:n], start=True, stop=True),
                         reads=[b_QFs[qs], b_nc], writes=[bankb[bk2]])
                    l0 = t0 - NCTX
                    P.op("pool", lambda e: e.tensor_tensor(out=t1b[:, 0:n], in0=qf[:, 0:n], in1=cosT[:, l0:l0 + n], op=ALU.mult),
                         reads=[b_QFs[qs], b_nc], writes=[b_T1s[qs]])
                    P.op("dve", lambda e: e.tensor_tensor(out=t2b[:, 0:n], in0=banks[bk2][:, 0:n], in1=sinT[:, l0:l0 + n], op=ALU.mult),
                         reads=[bankb[bk2], b_nc], writes=[b_T2s[qs]])
                    if which == 0:
                        P.op("dve", lambda e: e.tensor_tensor(out=QR[:, l0:l0 + n], in0=t1b[:, 0:n], in1=t2b[:, 0:n], op=ALU.add),
                             reads=[b_T1s[qs], b_T2s[qs]], writes=[b_QR])
                    else:
                        P.op("dve", lambda e: e.tensor_tensor(out=KR[:, t0:t0 + n], in0=t1b[:, 0:n], in1=t2b[:, 0:n], op=ALU.add),
                             reads=[b_T1s[qs], b_T2s[qs]], writes=[b_KR])

                qk_proj(0)
                for idx in range(len(qk_tiles)):
                    if idx + 1 < len(qk_tiles):
                        qk_proj(idx + 1)
                    qk_rope(idx)
                for hh in range(2):
                    h = 2 * hp + hh
                    if not (hh == 0 and hp > 0):
                        P.dma("sp", lambda e, h=h: e.dma_start(out=rpf, in_=rpbg_d[h]), writes=[b_rpf])
                    P.op("act", lambda e: e.activation(out=rpf, in_=rpf, func=AF.Exp), reads=[b_rpf], writes=[b_rpf])
                    P.op("pool", lambda e, hh=hh: e.tensor_tensor(out=expbv[:, :, hh, :], in0=rpf.rearrange("p (d c) -> p d c", d=8),
                                                                 in1=maskx.rearrange("p (d c) -> p d c", d=8), op=ALU.mult),
                         reads=[b_rpf, b_nc], writes=[b_expb[hh]])
                if hp + 1 < na_pairs:
                    P.dma("sp", lambda e: e.dma_start(out=rpf, in_=rpbg_d[2 * (hp + 1)]), writes=[b_rpf])
                bcolv = 4 * 8 + hp
                v_tiles = [(0, NCTX)] + ntiles(NCTX, TE)

                def v_proj(vi):
                    t0, t1 = v_tiles[vi]
                    n = t1 - t0
                    bk = vi % 2
                    vs = vi % 2
                    fns = [(lambda e, k=k: e.matmul(banks[bk][:, 0:n], lhsT=wv_[:, k, 256:384], rhs=xnTv[:, k, t0:t1],
                                                    start=(k == 0), stop=(k == 7))) for k in range(8)]
                    P.group("pe", fns, reads=[b_wq[sl]] + xn_bufs(t0, t1), writes=[bankb[bk]])
                    P.op("act", lambda e: e.activation(out=VTs[vs][:, 0:n], in_=banks[bk][:, 0:n], func=AF.Identity, bias=binpp[:, bcolv:bcolv + 1], scale=1.0),
                         reads=[bankb[bk], b_nc], writes=[b_VTs[vs]])

                def v_tr(vi):
                    t0, t1 = v_tiles[vi]
                    nt4 = (t1 - t0) // 128
                    vs = vi % 2
                    bk2 = 2 + (vi % 2)
                    bkv = banks[bk2].bitcast(BF16)
                    fns = [(lambda e, q=q: e.transpose(bkv[:, q * 128:(q + 1) * 128], VTs[vs][:, q * 128:(q + 1) * 128], identb)) for q in range(nt4)]
                    P.group("pe", fns, reads=[b_VTs[vs], b_const], writes=[bankb[bk2]])
                    ti0 = t0 // 128
                    P.op("dve", lambda e: e.tensor_copy(out=VEv[:, ti0:ti0 + nt4, :, 0:64],
                                                        in_=bkv[:, 0:nt4 * 128].rearrange("p (t h c) -> p t h c", t=nt4, h=2)),
                         reads=[bankb[bk2]], writes=[b_VE])

                v_proj(0)
                for vi in range(len(v_tiles)):
                    if vi + 1 < len(v_tiles):
                        v_proj(vi + 1)
                    v_tr(vi)
                P.dma("sp", lambda e: e.dma_start(out=VO[0:64, :], in_=VE[64:128, 2 * 130:33 * 130]), reads=[b_VE], writes=[b_VO])
                P.dma("sp", lambda e: e.dma_start(out=VO[64:128, :], in_=VE[0:64, 3 * 130:34 * 130]), reads=[b_VE], writes=[b_VO])

                ysl = hp % 2
                b_pvh = [Buf("pvh0"), Buf("pvh1")]
                b_th = [Buf("th0"), Buf("th1")]

                def emit_S(r):
                    rs = min(max(r - 4, 0), 56)
                    dl = r - rs
                    reg = r % 2
                    ebi = r % 3
                    fns = []
                    for kt in range(6):
                        for hh in range(2):
                            pb = hh * 64
                            k0 = NCTX + (rs + 2 * kt) * 64 if kt < 4 else (kt - 4) * 128
                            fns.append(lambda e, kt=kt, hh=hh, reg=reg, k0=k0, pb=pb, r=r: e.matmul(
                                banks[2 * reg + hh][:, kt * 64:(kt + 1) * 64], lhsT=KR[pb:pb + 64, k0:k0 + 128], rhs=QR[pb:pb + 64, r * 64:(r + 1) * 64],
                                start=True, stop=True))
                    P.group("pe", fns, reads=[b_KR, b_QR], writes=[bankb[2 * reg], bankb[2 * reg + 1]])
                    for hh in range(2):
                        P.op("act", lambda e, reg=reg, ebi=ebi, hh=hh: e.activation(out=EB[ebi][:, hh * 384:(hh + 1) * 384],
                                                                                  in_=banks[2 * reg + hh][:, 0:384], func=AF.Exp, scale=0.125),
                             reads=[bankb[2 * reg + hh]], writes=[b_EB[ebi]])
                    ebv = EB[ebi].rearrange("p (h c) -> p h c", h=2)
                    P.op("dve", lambda e, ebv=ebv, dl=dl: e.tensor_tensor(out=ebv[:, :, 0:256], in0=ebv[:, :, 0:256], in1=expbv[:, dl, :, :], op=ALU.mult),
                         reads=[b_EB[ebi], b_expb[0]], writes=[b_EB[ebi]])

                def emit_PV(r):
                    rs = min(max(r - 4, 0), 56)
                    reg = r % 3
                    pvb = 4 + (r % 2)
                    po = 0
                    fns = []
                    for hh in range(2):
                        for kt in range(6):
                            if kt < 4:
                                rv = VEv[:, 2 + rs // 2 + kt, hh, :] if rs % 2 == 0 else VOv[:, (rs - 1) // 2 + kt, hh, :]
                            else:
                                rv = VEv[:, kt - 4, hh, :]
                            fns.append(lambda e, kt=kt, rv=rv, reg=reg, hh=hh, po=po, pvb=pvb: e.matmul(
                                banks[pvb][0:64, po + hh * 128:po + hh * 128 + 65], lhsT=EB[reg][:, hh * 384 + kt * 64:hh * 384 + (kt + 1) * 64], rhs=rv,
                                start=(kt == 0), stop=(kt == 5)))
                    P.group("pe", fns, reads=[b_EB[reg], b_VE] + ([b_VO] if rs % 2 == 1 else []), writes=[bankb[pvb]])
                    pvv = banks[pvb][0:64, po:po + 256].rearrange("p (h c) -> p h c", h=2)
                    rdv = RD[0:64, (r % 4) * 2:(r % 4) * 2 + 2]
                    P.op("dve", lambda e, pvv=pvv, rdv=rdv: e.reciprocal(out=rdv, in_=pvv[:, :, 64]), reads=[bankb[pvb]], writes=[b_RD])
                    for hh in range(2):
                        P.op("dve", lambda e, pvv=pvv, rdv=rdv, hh=hh, r=r: e.tensor_scalar(
                            out=YT[r % 2][0:64, hh * 64:(hh + 1) * 64], in0=pvv[:, hh, 0:64], scalar1=rdv[:, hh:hh + 1], scalar2=None, op0=ALU.mult),
                            reads=[bankb[pvb], b_RD], writes=[b_YT[r % 2]])

                def emit_T(r):
                    tbk = 6 + (r % 2)
                    P.op("pe", lambda e, tbk=tbk, r=r: e.transpose(banks[tbk][:, 0:64], YT[r % 2][0:64, :], identf[0:64, 0:64]),
                         reads=[b_YT[r % 2], b_const], writes=[bankb[tbk]])
                    P.op("act", lambda e, tbk=tbk, r=r: e.activation(out=YN[ysl][:, r * 64:(r + 1) * 64], in_=banks[tbk][:, 0:64], func=AF.Identity),
                         reads=[bankb[tbk]], writes=[b_YN[ysl]])

                emit_S(0)
                for r in range(na_rows):
                    if r + 1 < na_rows:
                        emit_S(r + 1)
                    emit_PV(r)
                    if r >= 1:
                        emit_T(r - 1)
                emit_T(na_rows - 1)
                P.dma("sp", lambda e, ysl=ysl, hp=hp: e.dma_start(out=ynT_d[:, :, hp, :].rearrange("t p c -> p t c"), in_=YN[ysl].rearrange("p (t c) -> p t c", t=8)),
                      reads=[b_YN[ysl]], writes=[b_ynT])
            P.barrier()
            ar.reset(m0)

        pre3 = None
        if stage >= 4:
            Wr = ar.alloc_top(8 * D, BF16)
            Wn = ar.alloc_top(8 * D, BF16)
            Wo = ar.alloc_top(8 * D, BF16)
            modb3 = ar.alloc_top(4096)
            b_W = Buf("W3")
            b_modb = Buf("modb3")
            P.dma("sp", lambda e: e.dma_start(out=modb3, in_=modb_d), reads=[b_modbd], writes=[b_modb])
            Wrv = Wr.rearrange("p (k n) -> p k n", k=8)
            Wnv = Wn.rearrange("p (k n) -> p k n", k=8)
            Wov = Wo.rearrange("p (k n) -> p k n", k=8)
            for (dstv, src) in ((Wov, wout_d), (Wrv, wpr_d), (Wnv, wpn_d)):
                P.dma("pool", lambda e, dstv=dstv, src=src: e.dma_start(out=dstv, in_=src.rearrange("(k p) n -> p k n", p=128)), writes=[b_W])
            for k in range(8):
                P.op("dve", lambda e, k=k: e.tensor_tensor(out=Wov[:, k, :], in0=Wov[:, k, :], in1=modb3[:, 0:1024], op=ALU.mult),
                     reads=[b_W, b_modb], writes=[b_W])
            pre3 = True
        if stage >= 3:
            m0 = ar.mark()
            binpp = ar.alloc(56)
            b_c2c = Buf("c2c")
            P.dma("sp", lambda e: e.dma_start(out=binpp, in_=binpp_d), writes=[b_c2c])
            wgc = [ar.alloc(8 * 128, BF16), ar.alloc(8 * 128, BF16)]
            b_wgc = [Buf("wgc0"), Buf("wgc1")]
            SGB = [ar.alloc(NLAT, BF16), ar.alloc(NLAT, BF16)]
            b_SGB = [Buf("SGB0"), Buf("SGB1")]

            def load_gc(u):
                sl = u % 2
                wv = wgc[sl].rearrange("p (k n) -> p k n", k=8)
                P.dma("pool", lambda e: e.dma_start(out=wv, in_=win_v[:, :, 5 * D + u * 128:5 * D + (u + 1) * 128]), writes=[b_wgc[sl]])

            load_gc(8)
            u2 = 0
            for u in range(8, 16):
                sl = u % 2
                if u + 1 < 16:
                    load_gc(u + 1)
                wv = wgc[sl].rearrange("p (k n) -> p k n", k=8)
                for (t0, t1) in ntiles(NCTX, TE):
                    bk = u2 % 4
                    u2 += 1
                    fns = [(lambda e, k=k, bk=bk, t0=t0, t1=t1, wv=wv: e.matmul(banks[bk][:, 0:t1 - t0], lhsT=wv[:, k, :], rhs=xnTv[:, k, t0:t1],
                                                                             start=(k == 0), stop=(k == 7))) for k in range(8)]
                    P.group("pe", fns, reads=[b_wgc[sl]] + xn_bufs(t0, t1), writes=[bankb[bk]])
                    P.op("act", lambda e, bk=bk, t0=t0, t1=t1, sl=sl, u=u: e.activation(
                        out=SGB[sl][:, t0 - NCTX:t1 - NCTX], in_=banks[bk][:, 0:t1 - t0], func=AF.Sigmoid,
                        bias=binpp[:, 40 + u:41 + u], scale=1.0), reads=[bankb[bk], b_c2c], writes=[b_SGB[sl]])
                P.dma("sp", lambda e, sl=sl, u=u: e.dma_start(out=sg_d[u // 8, :, :, u % 8, :].rearrange("t p c -> p t c"), in_=SGB[sl].rearrange("p (t c) -> p t c", t=8)),
                      reads=[b_SGB[sl]], writes=[b_sgd])
            P.barrier()
            ar.reset(m0)

        if dbg and stage >= 2:
            d_yr = dout("d_yrT", [8, 128, 8, 512], BF16)
            P.dma("sp", lambda e: e.dma_start(out=d_yr, in_=yrT_d), reads=[b_yrT])
        if dbg and stage >= 3:
            d_yn = dout("d_ynT", [8, 128, 8, 512], BF16)
            P.dma("sp", lambda e: e.dma_start(out=d_yn, in_=ynT_d), reads=[b_ynT])
            d_sg = dout("d_sg", [2, 8, 128, 8, 512], BF16)
            P.dma("sp", lambda e: e.dma_start(out=d_sg, in_=sg_d), reads=[b_sgd])

        affT = None
        if stage >= 4:
            P.barrier()
            ar.reset(m_persist)
            affT = ar.alloc(32 * 16)
            b_aff = Buf("affT")
            m_aff = ar.mark()
            g1b = modb3[:, 0:1024]
            sh2b = modb3[:, 1024:2048]
            sc2b = modb3[:, 2048:3072]
            wrt = ar.alloc(128)
            b_c3 = Buf("c3")
            P.dma("sp", lambda e: e.dma_start(out=wrt, in_=wrt_d), writes=[b_c3])
            wrtv = wrt.rearrange("p (k n) -> p k n", k=8)
            yr = [ar.alloc(8 * 512, BF16), ar.alloc(8 * 512, BF16)]
            yn = [ar.alloc(8 * 512, BF16), ar.alloc(8 * 512, BF16)]
            sgr = [ar.alloc(8 * 512, BF16), ar.alloc(8 * 512, BF16)]
            sgn = [ar.alloc(8 * 512, BF16), ar.alloc(8 * 512, BF16)]
            b_yr = [Buf("yr0"), Buf("yr1")]
            b_yn = [Buf("yn0"), Buf("yn1")]
            b_sgr = [Buf("sgr0"), Buf("sgr1")]
            b_sgn = [Buf("sgn0"), Buf("sgn1")]
            M1 = ar.alloc(512)
            M2 = ar.alloc(512)
            b_M1, b_M2 = Buf("M1"), Buf("M2")
            MX = ar.alloc(8 * 512, BF16)
            b_MX = Buf("MX")
            MXv = MX.rearrange("p (k t) -> p k t", k=8)
            xt = [ar.alloc(1024), ar.alloc(1024)]
            b_xt = [Buf("xt0"), Buf("xt1")]
            x1 = [ar.alloc(1024), ar.alloc(1024)]
            b_x1 = [Buf("x10"), Buf("x11")]
            xq = [ar.alloc(1024) for _ in range(6)]
            b_xq = [Buf("xq%d" % i) for i in range(6)]
            xqb = [ar.alloc(1024, BF16), ar.alloc(1024, BF16)]
            b_xqb = [Buf("xqb0"), Buf("xqb1")]
            xqT = [ar.alloc(1024), ar.alloc(1024)]
            b_xqT = [Buf("xqT0"), Buf("xqT1")]
            junk = ar.alloc(1024)
            b_junk = Buf("junk3")
            yrT_v = yrT_d
            ynT_v = ynT_d
            sgr_v = sg_d[0]
            sgn_v = sg_d[1]
            stA = ar.alloc(16)
            stB = ar.alloc(16)
            b_stA = [Buf("stA%d" % i) for i in range(8)]
            b_stB = [Buf("stB%d" % i) for i in range(8)]

            def emit_A(tb, s_):
                ti = tb * 4 + s_
                xs = ti % 2
                xqs = ti % 6
                sa = stA[:, (ti % 8) * 2:(ti % 8) * 2 + 2]
                bsa = b_stA[ti % 8]
                P.dma("sp", lambda e: e.dma_start(out=xt[xs], in_=x_d[ti * 128:(ti + 1) * 128, :]), writes=[b_xt[xs]])
                for hf in range(2):
                    ob = 4 + hf
                    fns = [(lambda e, k=k, hf=hf, ob=ob: e.matmul(banks[ob][:, :], lhsT=MXv[:, k, s_ * 128:(s_ + 1) * 128],
                                                              rhs=Wov[:, k, hf * 512:(hf + 1) * 512], start=(k == 0), stop=(k == 7)))
                           for k in range(8)]
                    P.group("pe", fns, reads=[b_MX, b_W], writes=[bankb[ob]])
                    P.op("dve", lambda e, hf=hf, ob=ob: e.tensor_tensor(out=x1[xs][:, hf * 512:(hf + 1) * 512], in0=banks[ob][:, :],
                                                                     in1=xt[xs][:, hf * 512:(hf + 1) * 512], op=ALU.add),
                         reads=[bankb[ob], b_xt[xs]], writes=[b_x1[xs]])
                P.dma("pool", lambda e: e.dma_start(out=out_d[ti * 128:(ti + 1) * 128, :], in_=x1[xs]), reads=[b_x1[xs]], writes=[b_out])
                P.op("act", lambda e: e.activation(out=junk, in_=x1[xs], func=AF.Square, accum_out=sa[:, 0:1]),
                     reads=[b_x1[xs]], writes=[b_junk, bsa])
                P.op("act", lambda e: e.activation(out=sa[:, 1:2], in_=sa[:, 0:1], func=AF.Sqrt, bias=epsc[:, 0:1], scale=1.0 / D),
                     reads=[bsa, b_const], writes=[bsa])
                P.op("dve", lambda e: e.reciprocal(out=sa[:, 1:2], in_=sa[:, 1:2]), reads=[bsa], writes=[bsa])
                P.op("dve", lambda e: e.scalar_tensor_tensor(out=xq[xqs], in0=x1[xs], scalar=sa[:, 1:2], in1=sc2b, op0=ALU.mult, op1=ALU.mult),
                     reads=[b_x1[xs], bsa, b_modb], writes=[b_xq[xqs]])
                P.op("pool", lambda e: e.tensor_tensor(out=xq[xqs], in0=xq[xqs], in1=sh2b, op=ALU.add), reads=[b_xq[xqs], b_modb], writes=[b_xq[xqs]])
                P.op("act", lambda e: e.activation(out=xqb[xs], in_=xq[xqs], func=AF.Identity), reads=[b_xq[xqs]], writes=[b_xqb[xs]])
                P.dma("pool", lambda e: e.dma_start(out=xn2_d[ti * 128:(ti + 1) * 128, :], in_=xqb[xs]), reads=[b_xqb[xs]], writes=[b_xn2d])

            def emit_B(ti):
                xqs = ti % 6
                ts_ = ti % 2
                sb_ = stB[:, (ti % 8) * 2:(ti % 8) * 2 + 2]
                bsb = b_stB[ti % 8]
                xT = xqT[ts_]
                xTv = xT.rearrange("p (k t) -> p k t", k=8)
                for hb in range(2):
                    bk = 6 + hb
                    fns = [(lambda e, kk=kk, hb=hb, bk=bk: e.transpose(banks[bk][:, kk * 128:(kk + 1) * 128],
                                                                    xq[xqs][:, (hb * 4 + kk) * 128:(hb * 4 + kk + 1) * 128], identf))
                           for kk in range(4)]
                    P.group("pe", fns, reads=[b_xq[xqs], b_const], writes=[bankb[bk]])
                    if hb == 0:
                        P.op("act", lambda e, bk=bk: e.activation(out=xT[:, 0:512], in_=banks[bk][:, :], func=AF.Identity), reads=[bankb[bk]], writes=[b_xqT[ts_]])
                    else:
                        P.op("dve", lambda e, bk=bk: e.tensor_copy(out=xT[:, 512:1024], in_=banks[bk][:, :]), reads=[bankb[bk]], writes=[b_xqT[ts_]])
                fns = [(lambda e, k=k: e.matmul(banks[6][:, 0:16], lhsT=xTv[:, k, :], rhs=wrtv[:, k, :], start=(k == 0), stop=(k == 7))) for k in range(8)]
                P.group("pe", fns, reads=[b_xqT[ts_], b_c3], writes=[bankb[6]])
                P.op("act", lambda e: e.activation(out=affT[:, ti * 16:(ti + 1) * 16], in_=banks[6][:, 0:16], func=AF.Exp, accum_out=sb_[:, 0:1]),
                     reads=[bankb[6]], writes=[b_aff, bsb])
                P.op("dve", lambda e: e.reciprocal(out=sb_[:, 1:2], in_=sb_[:, 0:1]), reads=[bsb], writes=[bsb])
                P.op("dve", lambda e: e.tensor_scalar(out=affT[:, ti * 16:(ti + 1) * 16], in0=affT[:, ti * 16:(ti + 1) * 16], scalar1=sb_[:, 1:2],
                                                     scalar2=None, op0=ALU.mult), reads=[b_aff, bsb], writes=[b_aff])

            def load_blk(tb):
                sl = tb % 2
                T0 = tb * 512
                for (buf, bb, src, bsrc) in ((yr[sl], b_yr[sl], yrT_v, b_yrT), (yn[sl], b_yn[sl], ynT_v, b_ynT),
                                             (sgr[sl], b_sgr[sl], sgr_v, b_sgd), (sgn[sl], b_sgn[sl], sgn_v, b_sgd)):
                    bv = buf.rearrange("p (k t) -> p k t", k=8)
                    P.dma("sp", lambda e, bv=bv, src=src: e.dma_start(out=bv, in_=src[tb]), reads=[bsrc], writes=[bb])

            u3 = 0
            for tb in range(8):
                sl = tb % 2
                T0 = tb * 512
                yrv = yr[sl].rearrange("p (k t) -> p k t", k=8)
                ynv = yn[sl].rearrange("p (k t) -> p k t", k=8)
                srv = sgr[sl].rearrange("p (k t) -> p k t", k=8)
                snv = sgn[sl].rearrange("p (k t) -> p k t", k=8)
                if tb == 0:
                    load_blk(0)
                if tb + 1 < 8:
                    load_blk(tb + 1)
                for c in range(8):
                    for br in range(2):
                        ysrc = yrv if br == 0 else ynv
                        bys = b_yr[sl] if br == 0 else b_yn[sl]
                        gsrc = srv if br == 0 else snv
                        bgs = b_sgr[sl] if br == 0 else b_sgn[sl]
                        Wp = Wrv if br == 0 else Wnv
                        zb = 0 + (u3 % 4)
                        u3 += 1
                        fns = [(lambda e, k=k, zb=zb, Wp=Wp, ysrc=ysrc, c=c: e.matmul(banks[zb][:, :], lhsT=Wp[:, k, c * 128:(c + 1) * 128], rhs=ysrc[:, k, :],
                                                                                  start=(k == 0), stop=(k == 7))) for k in range(8)]
                        P.group("pe", fns, reads=[b_W, bys], writes=[bankb[zb]])
                        Mx = M1 if br == 0 else M2
                        bMx = b_M1 if br == 0 else b_M2
                        P.op("dve", lambda e, zb=zb, gsrc=gsrc, c=c, Mx=Mx: e.tensor_tensor(out=Mx, in0=banks[zb][:, :], in1=gsrc[:, c, :], op=ALU.mult),
                             reads=[bankb[zb], bgs], writes=[bMx])
                    P.op("pool", lambda e, c=c: e.tensor_tensor(out=MXv[:, c, :], in0=M1, in1=M2, op=ALU.add),
                         reads=[b_M1, b_M2], writes=[b_MX])
                    if tb >= 1 and c % 2 == 1:
                        emit_B((tb - 1) * 4 + c // 2)
                for s_ in range(4):
                    emit_A(tb, s_)
            for ti in range(28, 32):
                emit_B(ti)
            if dbg:
                d_aff = dout("d_affT", [128, 512])
                P.dma("sp", lambda e: e.dma_start(out=d_aff, in_=affT), reads=[b_aff])
            P.barrier()
            ar.reset(m_aff)
            ar.nwords = ar.total

        if stage >= 6:
            WG = [ar.alloc_top(8 * 1024, BF16), ar.alloc_top(8 * 1024, BF16)]
            WU = [ar.alloc_top(8 * 1024, BF16), ar.alloc_top(8 * 1024, BF16)]
            WD = [ar.alloc_top(8 * 1024, BF16), ar.alloc_top(8 * 1024, BF16), ar.alloc_top(8 * 1024, BF16)]
            b_WG = [Buf("WG0"), Buf("WG1")]
            b_WD = [Buf("WD%d" % i) for i in range(3)]

            def load_g(ex, half):
                sl = (ex * 2 + half) % 2
                wgv = WG[sl].rearrange("p (k n) -> p k n", k=8)
                wuv = WU[sl].rearrange("p (k n) -> p k n", k=8)
                P.dma("pool", lambda e: e.dma_start(out=wgv, in_=wg_d[ex].rearrange("(k p) f -> p k f", p=128)[:, :, half * 1024:(half + 1) * 1024]),
                      writes=[b_WG[sl]])
                P.dma("pool", lambda e: e.dma_start(out=wuv, in_=wu_d[ex].rearrange("(k p) f -> p k f", p=128)[:, :, half * 1024:(half + 1) * 1024]),
                      writes=[b_WG[sl]])

            def load_d(ex, half):
                sl = (ex * 2 + half) % 3
                wdv = WD[sl].rearrange("p (f n) -> p f n", f=8)
                P.dma("pool", lambda e: e.dma_start(out=wdv, in_=wd_d[ex].rearrange("(f p) n -> p f n", p=128)[:, half * 8:(half + 1) * 8, :]),
                      writes=[b_WD[sl]])

            load_g(0, 0)
            load_g(0, 1)
            load_d(0, 0)
            load_d(0, 1)
        if stage >= 5:
            m4 = ar.mark()
            idxf = ar.alloc(64)
            idxi = ar.alloc(64, I32)
            gsl = ar.alloc(64)
            b_idx = Buf("idx")
            m_idx = ar.mark()
            codeT = ar.alloc(512)
            b_code = Buf("codeT")
            cols = ar.alloc(32 * 16 * 5, BF16)
            b_cols = Buf("cols")
            colsv = cols.rearrange("p (t e c) -> p t e c", t=32, e=16)
            iot = ar.alloc(CAP, mybir.dt.uint16)
            wcomb = ar.alloc(2)
            PM = [ar.alloc(CAP, BF16) for _ in range(6)]
            b_PM = [Buf("PM%d" % i) for i in range(6)]
            RS = ar.alloc(CAP)
            b_RS = Buf("RS")
            m_keep = ar.mark()
            tokc = ar.alloc(1024, BF16)
            b_c4 = Buf("c4")
            P.dma("sp", lambda e: e.dma_start(out=tokc, in_=tokc_d), writes=[b_c4])
            P.dma("sp", lambda e: e.dma_start(out=iot, in_=iota_d), writes=[b_c4])
            P.dma("sp", lambda e: e.dma_start(out=wcomb[0:5, :], in_=wc_d), writes=[b_c4])
            tokcv = tokc.rearrange("p (t e c) -> p t e c", t=32, e=16)
            affTv = affT.rearrange("p (t e) -> p t e", e=16)
            AT = ar.alloc(512)
            A8 = ar.alloc(512)
            CM8 = ar.alloc(512)
            CU8 = ar.alloc(512)
            ON8 = ar.alloc(512)
            Gm = ar.alloc(128)
            Lm = ar.alloc(128)
            b8 = ar.alloc(16)
            b_AT, b_A8, b_CM8, b_CU8, b_ON8, b_GL, b_b8 = (Buf(n) for n in ("AT", "A8", "CM8", "CU8", "ON8", "GL", "b8"))
            P.dma("sp", lambda e: e.dma_start(out=Gm, in_=g8_d), writes=[b_GL])
            P.dma("sp", lambda e: e.dma_start(out=Lm, in_=l8_d), writes=[b_GL])
            lo8, mid8, tmp8 = b8[:, 0:1], b8[:, 1:2], b8[:, 4:5]
            cnt8 = b8[:, 2:4]
            P.op("dve", lambda e: e.memset(b8, 0.0), writes=[b_b8])
            P.op("pool", lambda e: e.memset(ON8, 1.0), writes=[b_ON8])
            P.op("dve", lambda e: e.tensor_copy(out=AT.rearrange("p (q e s) -> p q e s", q=4, e=16),
                                                in_=affT.rearrange("p (s q e) -> p q e s", s=8, q=4)), reads=[b_aff], writes=[b_AT])
            fns = [(lambda e, q=q: e.transpose(banks[0][:, q * 128:(q + 1) * 128], AT[:, q * 128:(q + 1) * 128], identf)) for q in range(4)]
            P.group("pe", fns, reads=[b_AT, b_const], writes=[bankb[0]])
            P.op("act", lambda e: e.activation(out=A8, in_=banks[0][:, :], func=AF.Identity), reads=[bankb[0]], writes=[b_A8])
            for it in range(25):
                wk = 2.0 ** (-(it + 1))
                P.op("dve", lambda e, wk=wk: e.tensor_scalar(out=mid8, in0=lo8, scalar1=wk, scalar2=None, op0=ALU.add), reads=[b_b8], writes=[b_b8])
                P.op("dve", lambda e: e.tensor_scalar(out=CM8, in0=A8, scalar1=mid8, scalar2=0.0, op0=ALU.is_ge, op1=ALU.add, accum_out=cnt8[:, 0:1]),
                     reads=[b_A8, b_b8], writes=[b_CM8, b_b8])
                P.op("pe", lambda e: e.matmul(banks[1][:, 0:2], lhsT=Gm, rhs=cnt8, start=True, stop=True), reads=[b_GL, b_b8], writes=[bankb[1]])
                P.op("dve", lambda e, wk=wk: e.tensor_scalar(out=tmp8, in0=banks[1][:, 0:1], scalar1=float(CAP) - 0.5, scalar2=wk, op0=ALU.is_ge, op1=ALU.mult),
                     reads=[bankb[1]], writes=[b_b8])
                P.op("dve", lambda e: e.tensor_tensor(out=lo8, in0=lo8, in1=tmp8, op=ALU.add), reads=[b_b8], writes=[b_b8])
            P.op("dve", lambda e: e.tensor_scalar(out=CM8, in0=A8, scalar1=lo8, scalar2=0.0, op0=ALU.is_ge, op1=ALU.add, accum_out=cnt8[:, 0:1]),
                 reads=[b_A8, b_b8], writes=[b_CM8, b_b8])
            P.op("dve", lambda e: e.tensor_tensor_scan(out=CU8, data0=ON8, data1=CM8, initial=0.0, op0=ALU.mult, op1=ALU.add),
                 reads=[b_CM8, b_ON8], writes=[b_CU8])
            P.op("pe", lambda e: e.matmul(banks[1][:, 0:2], lhsT=Lm, rhs=cnt8, start=True, stop=True), reads=[b_GL, b_b8], writes=[bankb[1]])
            P.op("dve", lambda e: e.tensor_copy(out=tmp8, in_=banks[1][:, 0:1]), reads=[bankb[1]], writes=[b_b8])
            P.op("dve", lambda e: e.scalar_tensor_tensor(out=CU8, in0=CU8, scalar=tmp8, in1=CM8, op0=ALU.add, op1=ALU.mult),
                 reads=[b_CU8, b_CM8, b_b8], writes=[b_CU8])
            P.op("dve", lambda e: e.tensor_scalar(out=CU8, in0=CU8, scalar1=-1.0, scalar2=None, op0=ALU.add), reads=[b_CU8], writes=[b_CU8])
            fns = [(lambda e, q=q: e.transpose(banks[2][:, q * 128:(q + 1) * 128], CU8[:, q * 128:(q + 1) * 128], identf)) for q in range(4)]
            P.group("pe", fns, reads=[b_CU8, b_const], writes=[bankb[2]])
            P.op("dve", lambda e: e.tensor_copy(out=codeT.rearrange("p (s q e) -> p q e s", s=8, q=4),
                                                in_=banks[2][:, :].rearrange("p (q e s) -> p q e s", q=4, e=16)), reads=[bankb[2]], writes=[b_code])
            codeTv = codeT.rearrange("p (t e) -> p t e", e=16)
            for c_ in range(2):
                P.op("dve", lambda e, c_=c_: e.tensor_copy(out=colsv[:, :, :, c_], in_=tokcv[:, :, :, c_]),
                     reads=[b_c4], writes=[b_cols])
            R1 = ar.alloc(512)
            R2 = ar.alloc(512)
            b_R = Buf("R12")
            R1v = R1.rearrange("p (t e) -> p t e", e=16)
            R2v = R2.rearrange("p (t e) -> p t e", e=16)
            P.op("dve", lambda e: e.tensor_copy(out=colsv[:, :, :, 2], in_=affTv), reads=[b_aff], writes=[b_cols])
            P.op("dve", lambda e: e.tensor_tensor(out=R1v, in0=affTv, in1=colsv[:, :, :, 2], op=ALU.subtract), reads=[b_aff, b_cols], writes=[b_R])
            P.op("dve", lambda e: e.tensor_copy(out=colsv[:, :, :, 3], in_=R1v), reads=[b_R], writes=[b_cols])
            P.op("dve", lambda e: e.tensor_tensor(out=R2v, in0=R1v, in1=colsv[:, :, :, 3], op=ALU.subtract), reads=[b_R, b_cols], writes=[b_R])
            P.op("dve", lambda e: e.tensor_copy(out=colsv[:, :, :, 4], in_=R2v), reads=[b_R], writes=[b_cols])
            idxfv = idxf.rearrange("p (e s) -> p e s", s=4)
            gslv = gsl.rearrange("p (e s) -> p e s", s=4)
            idxiv = idxi.rearrange("p (e s) -> p e s", s=4)
            b_idxe = [Buf("idx%d" % i) for i in range(NE)]

            def idx_dve(ex, ti):
                pm = (ex * 32 + ti) % 6
                P.op("dve", lambda e: e.tensor_scalar(out=PM[pm], in0=iot, scalar1=codeTv[:, ti, ex:ex + 1], scalar2=None, op0=ALU.is_equal),
                     reads=[b_c4, b_code], writes=[b_PM[pm]])

            def idx_pe(ex, ti):
                pm = (ex * 32 + ti) % 6
                P.op("pe", lambda e: e.matmul(banks[0][0:5, :], lhsT=colsv[:, ti, ex, :], rhs=PM[pm], start=(ti == 0), stop=(ti == 31)),
                     reads=[b_cols, b_PM[pm]], writes=[bankb[0]])

            def idx_fin(ex):
                P.op("act", lambda e: e.activation(out=RS[0:5, :], in_=banks[0][0:5, :], func=AF.Identity), reads=[bankb[0]], writes=[b_RS])
                fns = [(lambda e, s_=s_: e.matmul(banks[1][:, 2 * s_:2 * s_ + 2], lhsT=RS[0:5, s_ * 128:(s_ + 1) * 128], rhs=wcomb[0:5, :],
                                                  start=True, stop=True)) for s_ in range(4)]
                P.group("pe", fns, reads=[b_RS, b_c4], writes=[bankb[1]])
                b2v = banks[1][:, 0:8].rearrange("p (s c) -> p s c", c=2)
                P.op("dve", lambda e: e.tensor_copy(out=idxfv[:, ex, :], in_=b2v[:, :, 0]), reads=[bankb[1]], writes=[b_idxe[ex]])
                P.op("dve", lambda e: e.tensor_copy(out=gslv[:, ex, :], in_=b2v[:, :, 1]), reads=[bankb[1]], writes=[b_idxe[ex]])
                P.op("dve", lambda e: e.tensor_copy(out=idxiv[:, ex, :], in_=idxfv[:, ex, :]), reads=[b_idxe[ex]], writes=[b_idxe[ex]])

            n_pre = NE if stage < 6 else 2
            for ex in range(n_pre):
                for ti in range(32):
                    idx_dve(ex, ti)
                    idx_pe(ex, ti)
                idx_fin(ex)
            if dbg:
                d_idx = dout("d_idx", [128, 64])
                d_gsl = dout("d_gsl", [128, 64])
                d_code = dout("d_codeT", [128, 512])
                P.dma("sp", lambda e: e.dma_start(out=d_idx, in_=idxf), reads=b_idxe)
                P.dma("sp", lambda e: e.dma_start(out=d_gsl, in_=gsl), reads=b_idxe)
                P.dma("sp", lambda e: e.dma_start(out=d_code, in_=codeT), reads=[b_code])
            P.barrier()
            ar.reset(m_keep)

        if stage >= 6:
            g2b = ar.alloc(1024)
            b_modb = Buf("g2b")
            P.dma("sp", lambda e: e.dma_start(out=g2b, in_=modb_d[:, 3072:4096]), reads=[b_modbd], writes=[b_modb])
            XE = [ar.alloc(4 * D, BF16), ar.alloc(4 * D, BF16)]
            b_XE = [Buf("XE0"), Buf("XE1")]
            XT = [ar.alloc(8 * CAP, BF16), ar.alloc(8 * CAP, BF16)]
            b_XT = [Buf("XT0"), Buf("XT1")]
            HT = ar.alloc(16 * CAP, BF16)
            b_HT = Buf("HT")
            HTv = HT.rearrange("p (f t) -> p f t", f=16)
            SGx = [ar.alloc(CAP), ar.alloc(CAP)]
            b_SGx = [Buf("SGx0"), Buf("SGx1")]
            YE = [ar.alloc(D), ar.alloc(D)]
            b_YE = [Buf("YE0"), Buf("YE1")]

            def gather(ex):
                sl = ex % 2
                xev = XE[sl].rearrange("p (s d) -> p s d", s=4)
                for s_ in range(4):
                    P.dma("pool", lambda e, s_=s_: e.indirect_dma_start(out=xev[:, s_, :], out_offset=None, in_=xn2_d[:, :],
                                                                      in_offset=bass.IndirectOffsetOnAxis(ap=idxiv[:, ex, s_:s_ + 1], axis=0)),
                          reads=[b_xn2d, b_idxe[ex]], writes=[b_XE[sl]])

            gather(0)
            u5 = 0
            for ex in range(NE):
                sl = ex % 2
                xev = XE[sl].rearrange("p (s d) -> p s d", s=4)
                xtv = XT[sl].rearrange("p (k t) -> p k t", k=8)
                for k in range(8):
                    bk = k % 2
                    bkv = banks[bk].bitcast(BF16)
                    fns = [(lambda e, bkv=bkv, s_=s_, k=k: e.transpose(bkv[:, s_ * 128:(s_ + 1) * 128], xev[:, s_, k * 128:(k + 1) * 128], identb))
                           for s_ in range(4)]
                    P.group("pe", fns, reads=[b_XE[sl], b_const], writes=[bankb[bk]])
                    if k % 2 == 0:
                        P.op("act", lambda e, bkv=bkv, k=k: e.activation(out=xtv[:, k, :], in_=bkv[:, 0:512], func=AF.Identity), reads=[bankb[bk]], writes=[b_XT[sl]])
                    else:
                        P.op("dve", lambda e, bkv=bkv, k=k: e.tensor_copy(out=xtv[:, k, :], in_=bkv[:, 0:512]), reads=[bankb[bk]], writes=[b_XT[sl]])
                if ex + 1 < NE:
                    gather(ex + 1)
                for half in range(2):
                    gs = (ex * 2 + half) % 2
                    wgv = WG[gs].rearrange("p (k n) -> p k n", k=8)
                    wuv = WU[gs].rearrange("p (k n) -> p k n", k=8)
                    for fc in range(8):
                        f = half * 8 + fc
                        gb = 2 + (u5 % 2)
                        ub = 4 + (u5 % 2)
                        sgi = u5 % 2
                        u5 += 1
                        fns = [(lambda e, k=k, gb=gb, fc=fc, wgv=wgv: e.matmul(banks[gb][:, :], lhsT=wgv[:, k, fc * 128:(fc + 1) * 128], rhs=xtv[:, k, :],
                                                                           start=(k == 0), stop=(k == 7))) for k in range(8)]
                        P.group("pe", fns, reads=[b_WG[gs], b_XT[sl]], writes=[bankb[gb]])
                        fns = [(lambda e, k=k, ub=ub, fc=fc, wuv=wuv: e.matmul(banks[ub][:, :], lhsT=wuv[:, k, fc * 128:(fc + 1) * 128], rhs=xtv[:, k, :],
                                                                           start=(k == 0), stop=(k == 7))) for k in range(8)]
                        P.group("pe", fns, reads=[b_WG[gs], b_XT[sl]], writes=[bankb[ub]])
                        nxe = ex + 2
                        if nxe < NE:
                            idx_dve(nxe, 2 * f)
                            idx_dve(nxe, 2 * f + 1)
                        P.op("act", lambda e, gb=gb, sgi=sgi: e.activation(out=SGx[sgi], in_=banks[gb][:, :], func=AF.Silu), reads=[bankb[gb]], writes=[b_SGx[sgi]])
                        P.op("dve", lambda e, ub=ub, sgi=sgi, f=f: e.tensor_tensor(out=HTv[:, f, :], in0=banks[ub][:, :], in1=SGx[sgi], op=ALU.mult),
                             reads=[bankb[ub], b_SGx[sgi]], writes=[b_HT])
                        if nxe < NE:
                            idx_pe(nxe, 2 * f)
                            idx_pe(nxe, 2 * f + 1)
                    nx = ex * 2 + half + 2
                    if nx < 2 * NE:
                        load_g(nx // 2, nx % 2)
                if ex + 2 < NE:
                    idx_fin(ex + 2)
                for s_ in range(4):
                    ysl = u5 % 2
                    for hf in range(2):
                        ob = 6 + hf
                        fns = []
                        for f in range(16):
                            ds = (ex * 2 + f // 8) % 3
                            wdv = WD[ds].rearrange("p (f n) -> p f n", f=8)
                            fns.append(lambda e, f=f, ob=ob, wdv=wdv, s_=s_, hf=hf: e.matmul(
                                banks[ob][:, :], lhsT=HTv[:, f, s_ * 128:(s_ + 1) * 128], rhs=wdv[:, f % 8, hf * 512:(hf + 1) * 512],
                                start=(f == 0), stop=(f == 15)))
                        P.group("pe", fns, reads=[b_HT, b_WD[(ex * 2) % 3], b_WD[(ex * 2 + 1) % 3]], writes=[bankb[ob]])
                        P.op("dve", lambda e, ob=ob, ysl=ysl, hf=hf, s_=s_: e.scalar_tensor_tensor(
                            out=YE[ysl][:, hf * 512:(hf + 1) * 512], in0=banks[ob][:, :], scalar=gslv[:, ex, s_:s_ + 1],
                            in1=g2b[:, hf * 512:(hf + 1) * 512], op0=ALU.mult, op1=ALU.mult),
                            reads=[bankb[ob], b_idxe[ex], b_modb], writes=[b_YE[ysl]])
                    u5 += 1
                    P.dma("pool", lambda e, ysl=ysl, s_=s_: e.indirect_dma_start(
                        out=out_d[:, :], out_offset=bass.IndirectOffsetOnAxis(ap=idxiv[:, ex, s_:s_ + 1], axis=0),
                        in_=YE[ysl], in_offset=None, compute_op=ALU.add), reads=[b_YE[ysl], b_idxe[ex], b_out], writes=[b_out])
                if ex + 1 < NE:
                    load_d(ex + 1, 0)
                    load_d(ex + 1, 1)
            P.barrier()
            ar.reset(m_persist)
            ar.nwords = ar.total
            fnb = ar.alloc(D)
            b_fnb = Buf("fnb")
            P.dma("sp", lambda e: e.dma_start(out=fnb, in_=fnb_d), writes=[b_fnb])
            xo = [ar.alloc(D) for _ in range(4)]
            b_xo = [Buf("xo%d" % i) for i in range(4)]
            yo = [ar.alloc(D) for _ in range(4)]
            b_yo = [Buf("yo%d" % i) for i in range(4)]
            junk = ar.alloc(D)
            b_junk = Buf("junkf")
            sf = ar.alloc(64)
            b_sfl = [Buf("sf%d" % i) for i in range(8)]
            for ti in range(32):
                sl = ti % 4
                b_sf = b_sfl[ti % 8]
                P.dma("sp", lambda e, sl=sl, ti=ti: e.dma_start(out=xo[sl], in_=out_d[ti * 128:(ti + 1) * 128, :]), writes=[b_xo[sl]])
                P.op("act", lambda e, sl=sl, ti=ti: e.activation(out=junk, in_=xo[sl], func=AF.Square, accum_out=sf[:, ti:ti + 1]),
                     reads=[b_xo[sl]], writes=[b_junk, b_sf])
                P.op("act", lambda e, ti=ti: e.activation(out=sf[:, 32 + ti:33 + ti], in_=sf[:, ti:ti + 1], func=AF.Sqrt, bias=epsc[:, 0:1], scale=1.0 / D),
                     reads=[b_sf, b_const], writes=[b_sf])
                P.op("dve", lambda e, ti=ti: e.reciprocal(out=sf[:, 32 + ti:33 + ti], in_=sf[:, 32 + ti:33 + ti]), reads=[b_sf], writes=[b_sf])
                P.op("dve", lambda e, sl=sl, ti=ti: e.scalar_tensor_tensor(out=yo[sl], in0=xo[sl], scalar=sf[:, 32 + ti:33 + ti], in1=fnb,
                                                                        op0=ALU.mult, op1=ALU.mult), reads=[b_xo[sl], b_sf, b_fnb], writes=[b_yo[sl]])
                P.dma("pool", lambda e, sl=sl, ti=ti: e.dma_start(out=out_d[ti * 128:(ti + 1) * 128, :], in_=yo[sl]), reads=[b_yo[sl]], writes=[Buf("outf")])

        P.final_wait("sp")

        with nc.Block() as block:
            @block.tensor
            def _(e):
                P.replay("pe", e)

            @block.scalar
            def _(e):
                P.replay("act", e)

            @block.vector
            def _(e):
                P.replay("dve", e)

            @block.gpsimd
            def _(e):
                P.replay("pool", e)

            @block.sync
            def _(e):
                P.replay("sp", e)
    return nc, list(dbg_d.keys())


def _pp(v, nchunk):
    return np.ascontiguousarray(np.asarray(v, np.float32).reshape(nchunk, 128).T)


def _consts():
    c = {}
    c["ident_f"] = np.eye(128, dtype=np.float32)
    c["ident_b"] = np.eye(128, dtype=np.float32).astype(ml_dtypes.bfloat16)
    t = np.arange(NLAT)
    row = (t // 64).astype(np.float32)
    col = (t % 64).astype(np.float32)
    inv = (10000.0 ** (-np.arange(16, dtype=np.float32) / 16)).astype(np.float32)
    ang_r = row[:, None] * inv[None, :]
    ang_c = col[:, None] * inv[None, :]
    cos_t = np.zeros((128, NLAT), np.float32)
    sin_t = np.zeros((128, NLAT), np.float32)
    rperm = np.zeros((128, 128), np.float32)
    for p in range(128):
        hh, d = divmod(p, 64)
        grp, dd = divmod(d, 32)
        ang = ang_r if grp == 0 else ang_c
        fi = dd % 16
        cos_t[p] = np.cos(ang[:, fi])
        if dd < 16:
            sin_t[p] = -np.sin(ang[:, fi])
            partner = p + 16
        else:
            sin_t[p] = np.sin(ang[:, fi])
            partner = p - 16
        rperm[partner, p] = 1.0
    c["cos_t"] = cos_t
    c["sin_t"] = sin_t
    c["rperm"] = rperm.astype(ml_dtypes.bfloat16)
    c["iota_slot"] = np.broadcast_to(np.arange(CAP, dtype=np.uint16)[None, :], (128, CAP)).copy()
    tok = (np.arange(32)[None, :] * 128 + np.arange(128)[:, None])
    tc = np.stack([tok // 64, tok % 64], axis=-1).astype(np.float32)
    c["tokcols"] = np.broadcast_to(tc[:, :, None, :], (128, 32, 16, 2)).reshape(128, 1024).astype(ml_dtypes.bfloat16)
    pidx = np.arange(128)
    same = (pidx[:, None] // 8) == (pidx[None, :] // 8)
    c["g8"] = same.astype(np.float32)
    c["l8"] = (same & ((pidx[:, None] % 8) < (pidx[None, :] % 8))).astype(np.float32)
    c["sel2"] = np.stack([np.ones(128, np.float32), np.zeros(128, np.float32)])
    c["wcomb"] = np.array([[64, 0], [1, 0], [0, 1], [0, 1], [0, 1]], np.float32)
    kc = np.arange(64)[:, None]
    qc = np.arange(64)[None, :]
    cstart = np.clip(qc - 8, 0, 48)
    ok = ((kc >= cstart) & (kc < cstart + 16)).astype(np.float32)
    ok2 = np.concatenate([ok, ok], axis=0)
    c["maskx"] = np.broadcast_to(ok2[:, None, :], (128, 32, 64)).reshape(128, 2048).copy()
    return c


def _rpb_gather(rpb):
    p = np.arange(128)
    krm = p // 64
    kc = p % 64
    dl = np.arange(8)
    kt = np.arange(4)
    qc = np.arange(64)
    kr = 2 * kt[None, None, :, None] + krm[:, None, None, None]
    dr = np.clip(kr - dl[None, :, None, None] + 7, 0, 14)
    dc = np.clip(kc[:, None, None, None] - qc[None, None, None, :], -15, 15) + 15
    dr = np.broadcast_to(dr, (128, 8, 4, 64))
    dc = np.broadcast_to(dc, (128, 8, 4, 64))
    g = rpb[:, dr, dc]
    return np.ascontiguousarray(g.reshape(16, 128, 2048).astype(np.float32))


def prepare_inputs(inputs):
    f = lambda a: np.ascontiguousarray(np.asarray(a, dtype=np.float32))
    x = f(inputs["x"])
    c = f(inputs["c"])
    ctx = f(inputs["ctx"])
    c_ctx = f(inputs["c_ctx"])
    shared = {}
    shared["w_mod"] = f(inputs["w_mod"][0])
    bm = f(inputs["b_mod"][0])
    shared["bmod_row"] = np.stack([bm, bm])
    shared["bmod_pp"] = _pp(bm, 48)
    shared["w_in"] = f(inputs["w_in"][0])
    bi = f(inputs["b_in"][0])
    shared["bin_pp"] = _pp(bi, 56)
    shared["bin_v"] = np.broadcast_to(bi[4 * D:5 * D][None, :], (128, D)).copy()
    cw = f(inputs["conv_w"][0])
    shared["convw_pp"] = np.ascontiguousarray(cw.T.reshape(8, 128, 4).transpose(1, 0, 2).reshape(128, 32))
    shared["convb_pp"] = _pp(f(inputs["conv_b"][0]), 8)
    wa = f(inputs["lru_wa"][0])
    wi = f(inputs["lru_wi"][0])
    wbd = np.zeros((2, 2, 8, 128, 128), np.float32)
    for d_ in range(2):
        for a_, w in enumerate((wa, wi)):
            for j in range(8):
                wbd[d_, a_, j, 0:64, 0:64] = w[d_, 2 * j]
                wbd[d_, a_, j, 64:128, 64:128] = w[d_, 2 * j + 1]
    shared["lru_wbd"] = wbd
    ba = f(inputs["lru_ba"][0])
    bi_ = f(inputs["lru_bi"][0])
    lb = np.zeros((128, 2, 2, 8), np.float32)
    for d_ in range(2):
        lb[:, d_, 0, :] = _pp(ba[d_], 8)
        lb[:, d_, 1, :] = _pp(bi_[d_], 8)
    shared["lru_b_pp"] = lb.reshape(128, 32)
    lam = f(inputs["lru_lambda"][0])
    shared["lam_pp"] = np.stack([_pp(lam[0], 8), _pp(lam[1], 8)], axis=1).reshape(128, 16)
    shared["rpbg"] = _rpb_gather(f(inputs["na_rpb"][0]))
    shared["w_proj_rnn"] = f(inputs["w_proj_rnn"][0])
    shared["w_proj_na"] = f(inputs["w_proj_na"][0])
    shared["w_out"] = f(inputs["w_out"][0])
    wr = f(inputs["w_router"][0])
    shared["wr_pp"] = np.ascontiguousarray(wr.reshape(8, 128, 16).transpose(1, 0, 2).reshape(128, 128))
    shared["w_exp_gate"] = f(inputs["w_exp_gate"][0])
    shared["w_exp_up"] = f(inputs["w_exp_up"][0])
    shared["w_exp_down"] = f(inputs["w_exp_down"][0])
    shared["fn_b"] = np.broadcast_to(f(inputs["final_norm"])[None, :], (128, D)).copy()
    shared.update(_consts())
    in_maps = []
    for b in range(x.shape[0]):
        m = dict(shared)
        m["x"] = x[b]
        m["ctx"] = ctx[b]
        cpp = np.stack([_pp(c[b], 8), _pp(c_ctx, 8)], axis=-1)
        m["cpp"] = np.ascontiguousarray(cpp.reshape(128, 16))
        in_maps.append(m)
    return in_maps


def kernel(**inputs):
    in_maps = prepare_inputs(inputs)
    nc, _ = build_nc()
    res = run_bass_kernel_spmd(nc, in_maps, core_ids=list(range(len(in_maps))))
    out = np.stack([np.asarray(r["out"], dtype=np.float32) for r in res.results], axis=0)
    return out
```
